# Optimizing a Trainium2 kernel written in Bass

```python
import jax, jax.numpy as jnp
from jax import lax
import numpy as np

D_MODEL = 1024
BATCH = 4
SEQ = 8192
DEPTH = 1

GRID_W = 64
CTX_LEN = 256
HGRN_HEADS = 4
HGRN_DK = 128
HGRN_DV = 128
HGRN_WIDTH = HGRN_HEADS * HGRN_DK
HGRN_CHUNK = 64
SGU_GROUPS = 4
SGU_CHUNK = 128
SGU_WIDTH = 512
SGU_GROUP_DIM = SGU_WIDTH // SGU_GROUPS
ROWS_PER_SGU_CHUNK = SGU_CHUNK // GRID_W
N_EXPERTS = 16
EXPERT_FF = 2048
CAPACITY_FACTOR = 2
N_MOD = 6
EPS = 1e-6
IN_COLS = 5 * HGRN_WIDTH + 2 * SGU_WIDTH + 2 * D_MODEL

kernel_name = 'hybrid_hgrn2_sgu_ecmoe_dit_layer'


def rmsnorm(x, w):
    xf = x.astype(jnp.float32)
    y = xf * lax.rsqrt(jnp.mean(xf * xf, axis=-1, keepdims=True) + EPS)
    return (y * w.astype(jnp.float32)).astype(x.dtype)


def adaln(cond, w, b):
    m = jax.nn.silu(cond) @ w + b
    m = m.reshape((-1, 1, N_MOD * D_MODEL))
    return jnp.split(m, N_MOD, axis=-1)


def modulate(h, shift, scale):
    return h * (1 + scale) + shift


def split_in(p):
    sizes = (HGRN_WIDTH,) * 5 + (SGU_WIDTH,) * 2 + (D_MODEL,) * 2
    idx = []
    acc = 0
    for s in sizes[:-1]:
        acc += s
        idx.append(acc)
    return jnp.split(p, idx, axis=-1)


def to_heads(a):
    return a.reshape(a.shape[0], a.shape[1], HGRN_HEADS, HGRN_DK)


def hgrn_forget(z, lb):
    f = lb + (1 - lb) * jax.nn.sigmoid(z.astype(jnp.float32))
    return jnp.log(f), 1 - f


def hgrn_chunk_scan(q, k, logf, v, s0):
    B_, T, H, DK = q.shape
    DV = v.shape[-1]
    nc = T // HGRN_CHUNK

    def to_chunks(a):
        return a.astype(jnp.float32).reshape(B_, nc, HGRN_CHUNK, H, a.shape[-1]).transpose(1, 0, 3, 2, 4)

    qc, kc, fc, vc = to_chunks(q), to_chunks(k), to_chunks(logf), to_chunks(v)
    mask = jnp.tril(jnp.ones((HGRN_CHUNK, HGRN_CHUNK), dtype=bool))[None, None, :, :, None]

    def step(S, inp):
        qb, kb, fb, vb = inp
        b = jnp.cumsum(fb, axis=2)
        diff = b[:, :, :, None, :] - b[:, :, None, :, :]
        decay = jnp.exp(jnp.where(mask, diff, -jnp.inf))
        scores = jnp.einsum('bhik,bhijk->bhij', qb, decay * kb[:, :, None, :, :])
        o = jnp.einsum('bhij,bhjv->bhiv', scores, vb) + jnp.einsum('bhik,bhkv->bhiv', qb * jnp.exp(b), S)
        b_last = b[:, :, -1:, :]
        S_new = jnp.exp(b_last[:, :, 0, :])[..., None] * S + jnp.einsum('bhjk,bhjv->bhkv', kb * jnp.exp(b_last - b), vb)
        return S_new, o

    s_fin, o = lax.scan(step, s0.astype(jnp.float32), (qc, kc, fc, vc))
    o = o.transpose(1, 0, 3, 2, 4).reshape(B_, T, H, DV)
    return o, s_fin


def hgrn_final_state(k, logf, v):
    b = jnp.cumsum(logf.astype(jnp.float32), axis=1)
    w = k.astype(jnp.float32) * jnp.exp(b[:, -1:] - b)
    return jnp.einsum('bthk,bthv->bhkv', w, v.astype(jnp.float32))


def sgu(u, v, norm_w, w_s, b_s, n_chunks):
    B_, T, _ = u.shape
    u = jax.nn.gelu(u).reshape(B_, n_chunks, SGU_CHUNK, SGU_GROUPS, SGU_GROUP_DIM)
    v = jax.nn.gelu(v).reshape(B_, T, SGU_GROUPS, SGU_GROUP_DIM)
    v = rmsnorm(v, norm_w.reshape(SGU_GROUPS, SGU_GROUP_DIM))
    v = v.reshape(B_, n_chunks, SGU_CHUNK, SGU_GROUPS, SGU_GROUP_DIM)
    mixed = jnp.einsum('gts,bnsgc->bntgc', w_s, v) + b_s.T[:, :, None]
    return (u * mixed).reshape(B_, T, SGU_WIDTH)


def token_mixer(parts, lb_f, lb_b, hgrn_norm_w, sgu_norm_w, sgu_w, sgu_b,
                w_branch_a, w_branch_b, w_out, s0_f, s0_b, n_sgu_chunks):
    q_p, ff_p, fb_p, v_p, g_p, u_p, sv_p, ga_p, gb_p = parts
    q = jax.nn.silu(to_heads(q_p))
    v = to_heads(v_p)
    logf_f, k_f = hgrn_forget(to_heads(ff_p), lb_f)
    logf_b, k_b = hgrn_forget(to_heads(fb_p), lb_b)
    o_f, s_f = hgrn_chunk_scan(q, k_f, logf_f, v, s0_f)
    o_b, s_b = hgrn_chunk_scan(q[:, ::-1], k_b[:, ::-1], logf_b[:, ::-1], v[:, ::-1], s0_b)
    o = o_f + o_b[:, ::-1]
    o = rmsnorm(o, hgrn_norm_w.reshape(HGRN_HEADS, HGRN_DV)).astype(g_p.dtype)
    o = o.reshape(o.shape[0], o.shape[1], HGRN_WIDTH) * jax.nn.silu(g_p)
    y_a = o @ w_branch_a
    y_b = sgu(u_p, sv_p, sgu_norm_w, sgu_w, sgu_b, n_sgu_chunks) @ w_branch_b
    merged = jax.nn.sigmoid(ga_p) * y_a + jax.nn.sigmoid(gb_p) * y_b
    return merged @ w_out, s_f, s_b


def moe_expert_choice(h, router_w, w_gate, w_up, w_down):
    B_, T, _ = h.shape
    cap = CAPACITY_FACTOR * T // N_EXPERTS
    aff = jax.nn.softmax((h @ router_w).astype(jnp.float32), axis=-1)
    gate, idx = lax.top_k(jnp.swapaxes(aff, 1, 2), cap)
    bidx = jnp.arange(B_)[:, None, None]
    xs = h[bidx, idx]
    hid = jax.nn.silu(jnp.einsum('becd,edf->becf', xs, w_gate)) * jnp.einsum('becd,edf->becf', xs, w_up)
    ye = jnp.einsum('becf,efd->becd', hid, w_down) * gate[..., None].astype(h.dtype)
    return jnp.zeros_like(h).at[bidx, idx].add(ye)


def setup_inputs(seed: int = 0) -> dict:
    key = jax.random.key(seed)
    ks = jax.random.split(key, 24)
    f32 = jnp.float32
    nrm = lambda k, shape, s: jax.random.normal(k, shape, f32) * s
    return {
        'x': nrm(ks[0], (BATCH, SEQ, D_MODEL), 1.0),
        'c': nrm(ks[1], (BATCH, D_MODEL), 1.0),
        'ctx': nrm(ks[2], (BATCH, CTX_LEN, D_MODEL), 1.0),
        'c_ctx': nrm(ks[3], (D_MODEL,), 1.0),
        'ada_w': nrm(ks[4], (DEPTH, D_MODEL, N_MOD * D_MODEL), 0.02),
        'ada_b': nrm(ks[5], (DEPTH, N_MOD * D_MODEL), 0.02),
        'norm_mix_w': 1 + nrm(ks[6], (DEPTH, D_MODEL), 0.05),
        'norm_ffn_w': 1 + nrm(ks[7], (DEPTH, D_MODEL), 0.05),
        'w_in': nrm(ks[8], (DEPTH, D_MODEL, IN_COLS), D_MODEL ** -0.5),
        'hgrn_lb_logits': nrm(ks[9], (2, DEPTH + 1, HGRN_WIDTH), 0.5),
        'hgrn_norm_w': 1 + nrm(ks[10], (DEPTH, HGRN_WIDTH), 0.05),
        'sgu_norm_w': 1 + nrm(ks[11], (DEPTH, SGU_WIDTH), 0.05),
        'sgu_w': nrm(ks[12], (DEPTH, SGU_GROUPS, SGU_CHUNK, SGU_CHUNK), SGU_CHUNK ** -0.5),
        'sgu_b': 1 + nrm(ks[13], (DEPTH, SGU_GROUPS, SGU_CHUNK), 0.05),
        'w_branch_a': nrm(ks[14], (DEPTH, HGRN_WIDTH, D_MODEL), HGRN_WIDTH ** -0.5),
        'w_branch_b': nrm(ks[15], (DEPTH, SGU_WIDTH, D_MODEL), SGU_WIDTH ** -0.5),
        'w_out': nrm(ks[16], (DEPTH, D_MODEL, D_MODEL), D_MODEL ** -0.5),
        'router_w': nrm(ks[17], (DEPTH, D_MODEL, N_EXPERTS), D_MODEL ** -0.5),
        'expert_w_gate': nrm(ks[18], (DEPTH, N_EXPERTS, D_MODEL, EXPERT_FF), D_MODEL ** -0.5),
        'expert_w_up': nrm(ks[19], (DEPTH, N_EXPERTS, D_MODEL, EXPERT_FF), D_MODEL ** -0.5),
        'expert_w_down': nrm(ks[20], (DEPTH, N_EXPERTS, EXPERT_FF, D_MODEL), EXPERT_FF ** -0.5),
        'final_norm_w': 1 + nrm(ks[21], (D_MODEL,), 0.05),
    }


def reference(x, c, ctx, c_ctx, ada_w, ada_b, norm_mix_w, norm_ffn_w, w_in, hgrn_lb_logits,
              hgrn_norm_w, sgu_norm_w, sgu_w, sgu_b, w_branch_a, w_branch_b, w_out, router_w,
              expert_w_gate, expert_w_up, expert_w_down, final_norm_w):
    B_, N, _ = x.shape
    ROWS = N // GRID_W
    n_sgu_lat = ROWS // ROWS_PER_SGU_CHUNK
    n_sgu_ctx = ctx.shape[1] // SGU_CHUNK
    lb_all = jnp.cumsum(jax.nn.softmax(hgrn_lb_logits.astype(jnp.float32), axis=1), axis=1)
    for l in range(DEPTH):
        last = l == DEPTH - 1
        lb_f = lb_all[0, l].reshape(HGRN_HEADS, HGRN_DK)
        lb_b = lb_all[1, l].reshape(HGRN_HEADS, HGRN_DK)
        sh_m, sc_m, g_m, sh_f, sc_f, g_f = adaln(c, ada_w[l], ada_b[l])
        csh_m, csc_m, cg_m, csh_f, csc_f, cg_f = adaln(c_ctx, ada_w[l], ada_b[l])

        h_ctx = modulate(rmsnorm(ctx, norm_mix_w[l]), csh_m, csc_m)
        pc = split_in(h_ctx @ w_in[l])
        if last:
            v_c = to_heads(pc[3])
            logf_f, k_f = hgrn_forget(to_heads(pc[1]), lb_f)
            logf_b, k_b = hgrn_forget(to_heads(pc[2]), lb_b)
            s_f = hgrn_final_state(k_f, logf_f, v_c)
            s_b = hgrn_final_state(k_b[:, ::-1], logf_b[:, ::-1], v_c[:, ::-1])
        else:
            zeros = jnp.zeros((ctx.shape[0], HGRN_HEADS, HGRN_DK, HGRN_DV), jnp.float32)
            y_c, s_f, s_b = token_mixer(pc, lb_f, lb_b, hgrn_norm_w[l], sgu_norm_w[l], sgu_w[l], sgu_b[l],
                                        w_branch_a[l], w_branch_b[l], w_out[l], zeros, zeros, n_sgu_ctx)
            ctx = ctx + cg_m * y_c
            h2c = modulate(rmsnorm(ctx, norm_ffn_w[l]), csh_f, csc_f)
            ctx = ctx + cg_f * moe_expert_choice(h2c, router_w[l], expert_w_gate[l], expert_w_up[l], expert_w_down[l])

        h = modulate(rmsnorm(x, norm_mix_w[l]), sh_m, sc_m)
        pl = split_in(h @ w_in[l])
        y, _, _ = token_mixer(pl, lb_f, lb_b, hgrn_norm_w[l], sgu_norm_w[l], sgu_w[l], sgu_b[l],
                              w_branch_a[l], w_branch_b[l], w_out[l], s_f, s_b, n_sgu_lat)
        x = x + g_m * y
        h2 = modulate(rmsnorm(x, norm_ffn_w[l]), sh_f, sc_f)
        x = x + g_f * moe_expert_choice(h2, router_w[l], expert_w_gate[l], expert_w_up[l], expert_w_down[l])
    return rmsnorm(x, final_norm_w)
```

```python
import numpy as np
from contextlib import ExitStack
import concourse.bass as bass
import concourse.mybir as mybir
from concourse.bass_utils import run_bass_kernel_spmd

F32 = mybir.dt.float32
BF16 = mybir.dt.bfloat16
I32 = mybir.dt.int32
AF = mybir.ActivationFunctionType
ALU = mybir.AluOpType
AX = mybir.AxisListType

D = 1024
KD = 8
HW = 512
NH = 4
INC = 5632
NE = 16
FF = 2048
CTX = 256
EPS = 1e-6
GELU_C = 1.5957691216057308


class Buf:
    __slots__ = ("name", "t", "w", "r", "dsem", "multi", "dw", "dr")

    def __init__(self, name, t=None, multi=False):
        self.name = name
        self.t = t
        self.w = []
        self.r = {}
        self.dsem = None
        self.multi = multi
        self.dw = []
        self.dr = []

    def __getitem__(self, k):
        return self.t[k]


class DmaSem:
    def __init__(self, h):
        self.h = h
        self.val = 0


class FW:
    SAME_ENGINE_SYNC = ("act", "dve", "pool")
    PARTIAL_OK = False
    XLAT = 150.0
    TBL_NS = 1280.0

    def __init__(self, nc):
        self.nc = nc
        self.eng = {"pe": nc.tensor, "act": nc.scalar, "dve": nc.vector,
                    "pool": nc.gpsimd, "sp": nc.sync}
        self.psem = {k: nc.alloc_semaphore(name="p_" + k) for k in ("pe", "act", "dve", "pool")}
        self.seq = {k: 0 for k in self.psem}
        self.waited = {k: {} for k in self.eng}
        self.n_wait = 0
        self.n_ins = 0
        self.banks = []
        self.bank_i = 0
        self.defer = False
        self.recs = []
        self.touched = []

    def sb(self, name, shape, dt, dma=False, stack=None):
        if stack is None:
            t = self.nc.alloc_sbuf_tensor("s_" + name, list(shape), dt)
        else:
            t = stack.enter_context(self.nc.sbuf_tensor("s_" + name, list(shape), dt))
        b = Buf(name, t)
        if dma is True:
            b.dsem = self.new_dsem(name)
        elif dma:
            b.dsem = dma
        return b

    def token(self, name):
        return Buf(name, None, multi=True)

    def new_dsem(self, name):
        return DmaSem(self.nc.alloc_semaphore(name="d_" + name))

    def init_banks(self):
        for i in range(8):
            t = self.nc.alloc_psum_tensor("bank%d" % i, [128, 512], F32)
            self.banks.append(Buf("bank%d" % i, t))

    def bank(self):
        b = self.banks[self.bank_i % 8]
        self.bank_i += 1
        return b

    def _semh(self, key):
        return self.psem[key] if isinstance(key, str) else key.h

    def _wait(self, e, key, val):
        if val <= 0:
            return
        if isinstance(key, str) and key == e and e not in self.SAME_ENGINE_SYNC:
            return
        w = self.waited[e]
        if w.get(key, 0) >= val:
            return
        w[key] = val
        self.eng[e].wait_ge(self._semh(key), val)
        self.n_wait += 1

    def _deps(self, e, reads, writes, skip=None, partial=False, same=False):
        for b in reads:
            for (k, v) in b.w:
                self._wait(e, k, v)
        for b in writes:
            if not partial:
                for (k, v) in b.w:
                    if k is not skip and not (same and k == e):
                        self._wait(e, k, v)
            for k, v in b.r.items():
                self._wait(e, k, v)

    def _commit(self, ev, reads, writes, partial=False):
        k, v = ev
        for b in writes:
            if b.multi or partial:
                b.w = [x for x in b.w if x[0] != k] + [ev]
            else:
                b.w = [ev]
            if not partial:
                b.r = {}
        for b in reads:
            if b in writes:
                continue
            if b.r.get(k, 0) < v:
                b.r[k] = v

    def begin_defer(self):
        self.defer = True
        self.recs = []
        self.touched = []

    @staticmethod
    def _nfree(ap):
        try:
            n = 1
            for d in ap.shape[1:]:
                n *= int(d)
            return n
        except Exception:
            return 512

    def _est(self, e, name, a, kw):
        ap = kw.get("out", a[0] if a else None)
        n = self._nfree(ap) if ap is not None else 512
        if e == "pe":
            return 110.0 if name == "transpose" else 70.0 + 0.42 * n
        if e == "act":
            return 230.0 + 0.75 * n
        if e == "dve":
            return 200.0 + 0.95 * n
        return 300.0 + 1.9 * n

    def _record(self, e, fn, reads, writes, dur, sem=None, lat=0.0, extra=(), nowaw=False, tbl=None, same=False):
        R = [len(self.recs), e, fn, None, [], dur, sem, None, lat, tbl]
        deps = set()
        ext = R[4]
        for b in extra:
            deps.update(b.dw)
            ext.extend(b.w)
        for b in reads:
            deps.update(b.dw)
            ext.extend(b.w)
        for b in writes:
            if same:
                deps.update(d for d in b.dw if self.recs[d][1] != e)
                ext.extend(b.w)
            elif not (b.multi or nowaw):
                deps.update(b.dw)
                ext.extend(b.w)
            deps.update(b.dr)
            ext.extend(b.r.items())
        for b in writes:
            if b.multi or nowaw or same:
                b.dw.append(R[0])
            else:
                b.dw = [R[0]]
                b.w = []
                b.r = {}
            b.dr = []
            self.touched.append(b)
        for b in reads:
            if b not in writes:
                b.dr.append(R[0])
                self.touched.append(b)
        R[3] = sorted(deps)
        self.recs.append(R)
        return None

    def flush(self, window=48):
        recs = self.recs
        self.defer = False
        n = len(recs)
        if n == 0:
            return
        engs = ("pe", "act", "dve", "pool", "sp")
        XLAT = self.XLAT
        TBL = self.TBL_NS
        ndep = [len(r[3]) for r in recs]
        users = [[] for _ in range(n)]
        for r in recs:
            for d in r[3]:
                users[d].append(r[0])
        rdy = [0.0] * n
        fin = [0.0] * n
        start = [0.0] * n
        ready = {e: [] for e in engs}
        for r in recs:
            if ndep[r[0]] == 0:
                ready[r[1]].append(r[0])
        etime = {e: 0.0 for e in engs}
        cur_tbl = None
        order = []
        remaining = n
        sched = [False] * n
        low = 0
        LOOK = window
        while remaining:
            best = None
            while low < n and sched[low]:
                low += 1
            lim = low + LOOK
            for e in engs:
                lst = ready[e]
                if not lst:
                    continue
                et = etime[e]
                for rid in lst:
                    if rid >= lim:
                        continue
                    st = rdy[rid] if rdy[rid] > et else et
                    if e == "act":
                        tb = recs[rid][9]
                        if tb is not None and tb != cur_tbl:
                            st += TBL
                    key = (st, rid)
                    if best is None or key < best[0]:
                        best = (key, e, rid, st)
            assert best is not None, "list scheduler stuck"
            _, e, rid, st = best
            ready[e].remove(rid)
            sched[rid] = True
            start[rid] = st
            r = recs[rid]
            if r[9] is not None:
                cur_tbl = r[9]
            etime[e] = st + r[5]
            f_ = st + r[5] + r[8]
            fin[rid] = f_
            for u in users[rid]:
                fu = f_ if recs[u][1] == e else f_ + XLAT
                if fu > rdy[u]:
                    rdy[u] = fu
                ndep[u] -= 1
                if ndep[u] == 0:
                    ready[recs[u][1]].append(u)
            order.append(rid)
            remaining -= 1
        order.sort(key=lambda rid: (start[rid], rid))
        self.sched_span = max(fin) if fin else 0.0
        for rid in order:
            r = recs[rid]
            e = r[1]
            for d in r[3]:
                k, v = recs[d][7]
                self._wait(e, k, v)
            for (k, v) in r[4]:
                self._wait(e, k, v)
            ins = r[2]()
            if r[6] is None:
                self.seq[e] += 1
                ins.then_inc(self.psem[e], 1)
                r[7] = (e, self.seq[e])
            else:
                sem = r[6]
                sem.val += 16
                ins.then_inc(sem.h, 16)
                r[7] = (sem, sem.val)
            self.n_ins += 1
        seen = set()
        for b in self.touched:
            if id(b) in seen:
                continue
            seen.add(id(b))
            evs = {}
            for d in b.dw:
                k, v = recs[d][7]
                if evs.get(k, 0) < v:
                    evs[k] = v
            if b.dw:
                if b.multi:
                    old = {k: v for (k, v) in b.w}
                    old.update(evs)
                    b.w = list(old.items())
                else:
                    b.w = list(evs.items())
            for d in b.dr:
                k, v = recs[d][7]
                if b.r.get(k, 0) < v:
                    b.r[k] = v
            b.dw = []
            b.dr = []
        self.recs = []
        self.touched = []

    def op(self, e, name, reads, writes, *a, **kw):
        if getattr(self, "defer", False):
            kw.pop("partial", None)
            same = kw.pop("same_slice", False)
            eng = self.eng[e]
            fn = (lambda eng=eng, name=name, a=a, kw=kw: getattr(eng, name)(*a, **kw))
            tbl = None
            if e == "act":
                f_ = kw.get("func")
                if f_ == AF.Tanh:
                    tbl = "T"
                elif f_ == AF.Ln:
                    tbl = "L"
            return self._record(e, fn, list(reads), list(writes), self._est(e, name, a, kw), tbl=tbl, same=same)
        partial = kw.pop("partial", False) and self.PARTIAL_OK
        same = kw.pop("same_slice", False)
        self._deps(e, reads, writes, partial=partial, same=same)
        ins = getattr(self.eng[e], name)(*a, **kw)
        self.seq[e] += 1
        ins.then_inc(self.psem[e], 1)
        self._commit((e, self.seq[e]), reads, writes, partial=partial)
        self.n_ins += 1
        return ins

    def dma(self, q, out, in_, reads=(), writes=(), sem=None, indirect=None, extra=(), nowait_w=False, **kw):
        if getattr(self, "defer", False):
            if sem is None:
                for b in list(writes) + list(reads):
                    if b.dsem is not None:
                        sem = b.dsem
                        break
            assert sem is not None, "dma needs a semaphore"
            eng = self.eng[q]
            if indirect is not None:
                fn = (lambda: eng.indirect_dma_start(out=out, in_=in_, **indirect))
            else:
                fn = (lambda: eng.dma_start(out=out, in_=in_, **kw))
            nbytes = 4.0 * self._nfree(out) * 128
            return self._record(q, fn, list(reads), list(writes), 120.0, sem=sem, lat=2500.0 + nbytes / 150.0,
                                extra=extra, nowaw=nowait_w)
        for b in extra:
            for (k, v) in b.w:
                self._wait(q, k, v)
        if sem is None:
            for b in list(writes) + list(reads):
                if b.dsem is not None:
                    sem = b.dsem
                    break
        assert sem is not None, "dma needs a semaphore"
        self._deps(q, reads, writes, skip=sem, partial=nowait_w)
        if indirect is not None:
            ins = self.eng[q].indirect_dma_start(out=out, in_=in_, **indirect)
        else:
            ins = self.eng[q].dma_start(out=out, in_=in_, **kw)
        sem.val += 16
        ins.then_inc(sem.h, 16)
        self._commit((sem, sem.val), reads, writes)
        self.n_ins += 1
        return ins

    def finish(self, bufs):
        for b in bufs:
            for (k, v) in b.w:
                self._wait("sp", k, v)


C_IDENT, C_MF, C_MB, C_MASKF, C_MASKB, C_SELF, C_SELB, C_SEL, C_IOSI, C_IOSB, C_PV, C_JV, C_ONES, C_LSTR = \
    0, 128, 256, 384, 512, 640, 642, 644, 900, 1028, 1036, 1037, 1101, 1229
NCST = 1357


def make_consts():
    c = np.zeros((128, NCST), np.float32)
    s = np.arange(128)[:, None]
    t = np.arange(128)[None, :]
    c[:, C_IDENT:C_IDENT + 128] = np.eye(128)
    mf = np.where((t >= 64) & (s >= 64) & (s <= t), 1.0, 0.0) - np.where((t < 64) & (s > t) & (s <= 63), 1.0, 0.0)
    mb = np.where((t < 64) & (s >= t) & (s <= 63), 1.0, 0.0) - np.where((t >= 64) & (s >= 64) & (s < t), 1.0, 0.0)
    c[:, C_MF:C_MF + 128] = mf
    c[:, C_MB:C_MB + 128] = mb
    c[:, C_MASKF:C_MASKF + 128] = (s <= t)
    c[:, C_MASKB:C_MASKB + 128] = (s >= t)
    c[:, C_SELF] = (np.arange(128) <= 63)
    c[:, C_SELF + 1] = 1.0
    c[:, C_SELB] = (np.arange(128) >= 64)
    c[:, C_SELB + 1] = 1.0
    c[0, C_SEL:C_SEL + 128] = 1.0
    c[1, C_SEL + 128:C_SEL + 256] = 1.0
    c[:, C_IOSI:C_IOSI + 128] = np.arange(128)[None, :]
    c[:, C_IOSB:C_IOSB + 8] = np.arange(8)[None, :]
    c[:, C_PV] = np.arange(128) + 1
    c[:, C_JV:C_JV + 64] = np.arange(64)[None, :]
    c[:, C_ONES:C_ONES + 128] = 1.0
    c[:, C_LSTR:C_LSTR + 128] = (s < t)
    return c


def build(NT=64, debug=None, n_bisect=30, NSA=3, NSC=4, WIN=150):
    T = NT * 128
    CAP = 2 * T // NE
    SB = CAP // 128
    assert SB >= 1
    nc = bass.Bass("TRN2", target_bir_lowering=False)
    dt = lambda n, s, d=F32, kind="ExternalInput": nc.dram_tensor(n, list(s), d, kind=kind)
    x_d = dt("x", [T, D])
    ctx_d = dt("ctx", [CTX, D])
    cc_d = dt("ccT", [128, KD, 2])
    adaw_d = dt("ada_w", [D, 6 * D])
    adab_d = dt("ada_b", [1, 6 * D])
    nw_d = dt("nw", [3, D])
    win_d = dt("w_in", [D, INC])
    lbl_d = dt("lbl", [1, 2 * 2 * HW])
    hnw_d = dt("hnw", [1, HW])
    snw_d = dt("snw", [1, HW])
    sguw_d = dt("sgu_wT", [128, 4, 128])
    sgub_d = dt("sgu_bT", [128, 4])
    wa_d = dt("w_a", [HW, D])
    wb_d = dt("w_b", [HW, D])
    wo_d = dt("w_o", [D, D])
    rw_d = dt("rw", [D, NE])
    if debug not in ("xmid", "route"):
        wg_l = [dt("wg%d" % e, [D, FF]) for e in range(NE)]
        wu_l = [dt("wu%d" % e, [D, FF]) for e in range(NE)]
        wd_l = [dt("wd%d" % e, [FF, D]) for e in range(NE)]
    cst_d = dt("cst", [128, NCST])
    out_d = dt("out", [T, D], F32, "ExternalOutput")
    if debug == "xmid":
        xacc_d = dt("xacc", [T, D], F32, "ExternalOutput")
        h2_d = dt("h2d", [T, D], BF16, "ExternalOutput")
        aff_dbg = dt("affd", [128, NT, NE], F32, "ExternalOutput")
    else:
        xacc_d = nc.dram_tensor("xacc", [T, D], F32)
        h2_d = nc.dram_tensor("h2d", [T, D], BF16)
    if debug == "route":
        aff_dbg = dt("affd", [128, NT, NE], F32, "ExternalOutput")
        idx_dbg = dt("idxd", [128, NE, SB], I32, "ExternalOutput")
        gate_dbg = dt("gated", [128, NE, SB], F32, "ExternalOutput")
    ob_d = nc.dram_tensor("obd", [T, HW], BF16)

    mods_d = nc.dram_tensor("mods", [2, 6, D], F32)
    fw = FW(nc)
    fw.init_banks()
    op = fw.op
    for bk_ in fw.banks:
        op("dve", "memset", [], [bk_], bk_[:, :], 0.0)
    S0 = ExitStack()
    S1 = ExitStack()

    setup_sems = {"hw": fw.new_dsem("setup"), "pool": fw.new_dsem("setup_sw")}
    setup_bufs = []

    def sload(q, name, shape, dtp, src, stack=S0):
        sem_ = setup_sems["pool" if q == "pool" else "hw"]
        b = fw.sb(name, shape, dtp, dma=sem_, stack=stack)
        fw.dma(q, b[:], src, writes=[b])
        setup_bufs.append(b)
        return b

    sf_count = [0]

    def sfinal():
        for b in setup_bufs:
            b.w = [(b.dsem, b.dsem.val)]
        del setup_bufs[:]
        sf_count[0] += 1
        setup_sems["hw"] = fw.new_dsem("setup%d" % sf_count[0])
        setup_sems["pool"] = fw.new_dsem("setup_sw%d" % sf_count[0])

    NCA = C_SEL
    cst = sload("sp", "cst", [128, NCA], F32, cst_d[:, 0:NCA])
    identb = sload("pool", "identb", [128, 128], BF16, cst_d[:, C_IDENT:C_IDENT + 128])
    maski = sload("pool", "maski", [128, 2, 128], I32,
                  cst_d[:, C_MASKF:C_MASKF + 256].rearrange("p (a b) -> p a b", a=2))
    mhalf = fw.sb("mhalf", [128, 8], F32, stack=S0)
    fw.op("pool", "memset", [], [mhalf], mhalf[:], -0.5)
    AFF = fw.sb("AFF", [128, NT, NE], F32, stack=S0)
    RS = fw.sb("RS", [128, NT], F32, stack=S0)
    lb = fw.sb("lb", [128, 2, HW], F32, stack=S0)
    oml = fw.sb("oml", [128, 2, HW], F32, stack=S0)
    colv = fw.sb("colv", [128, 2, 6, KD], F32, dma=True, stack=S0)
    ident = cst[:, C_IDENT:C_IDENT + 128]
    mods_tok = fw.token("mods_tok")

    NA = 3584
    win = fw.sb("winA", [128, KD, NA], BF16, dma=True, stack=S1)
    for k in range(KD):
        fw.dma("pool", win[:, k, :], win_d[k * 128:(k + 1) * 128, 0:NA], writes=[win])
    with ExitStack() as SA:
        lbl = sload("sp", "lbl", [128, 2, 2, HW], F32,
                    lbl_d.ap().to_broadcast([128, 2 * 2 * HW]).rearrange("p (a b c) -> p a b c", a=2, b=2), stack=SA)
        nw2 = sload("sp", "nw2", [2, 2, D], F32,
                    nw_d[0:2, :].rearrange("(o a) d -> o a d", o=1).to_broadcast([2, 2, D]), stack=SA)
        adab2 = sload("sp", "adab2", [2, 6 * D], F32, adab_d.ap().to_broadcast([2, 6 * D]), stack=SA)
        ccT = sload("sp", "ccT", [128, KD, 2], F32, cc_d.ap(), stack=SA)
        sfinal()
        op("dve", "tensor_tensor", [lbl], [lb], out=lb[:], in0=lbl[:, :, 0, :], in1=lbl[:, :, 1, :], op=ALU.subtract)
        op("act", "activation", [lb], [lb], out=lb[:], in_=lb[:], func=AF.Sigmoid)
        op("dve", "tensor_scalar", [lb], [oml], out=oml[:], in0=lb[:], scalar1=-0.5, scalar2=0.5,
           op0=ALU.mult, op1=ALU.add)
        op("dve", "tensor_tensor", [lb, oml], [lb], out=lb[:], in0=lb[:], in1=oml[:], op=ALU.add)
        scc = fw.sb("scc", [128, KD, 2], F32, stack=SA)
        msb = fw.sb("msb", [2, 6 * D], F32, stack=SA)
        vec = fw.sb("vec", [2, 6, D], F32, dma=True, stack=SA)
        awb = [fw.sb("awb%d" % i, [128, KD, 512], F32, dma=True, stack=SA) for i in range(2)]
        op("act", "activation", [ccT], [scc], out=scc[:], in_=ccT[:], func=AF.Sigmoid)
        op("dve", "tensor_tensor", [scc, ccT], [scc], out=scc[:], in0=scc[:], in1=ccT[:], op=ALU.mult)
        for n in range(12):
            a = awb[n % 2]
            fw.dma("sp" if n % 2 == 0 else "act", a[:],
                   adaw_d[:, n * 512:(n + 1) * 512].rearrange("(k p) n -> p k n", p=128), writes=[a])
            bk = fw.bank()
            for k in range(KD):
                op("pe", "matmul", [scc, a], [bk], bk[0:2, :], lhsT=scc[:, k, :], rhs=a[:, k, :],
                   start=(k == 0), stop=(k == KD - 1))
            op("dve", "tensor_tensor", [bk, adab2], [msb], out=msb[:, n * 512:(n + 1) * 512], in0=bk[0:2, :],
               in1=adab2[:, n * 512:(n + 1) * 512], op=ALU.add)
        op("dve", "scalar_tensor_tensor", [msb, nw2], [vec], out=vec[:, 0, :], in0=msb[:, D:2 * D], scalar=1.0,
           in1=nw2[:, 0, :], op0=ALU.add, op1=ALU.mult)
        op("dve", "tensor_copy", [msb], [vec], out=vec[:, 1, :], in_=msb[:, 0:D])
        op("dve", "tensor_copy", [msb], [vec], out=vec[:, 2, :], in_=msb[:, 2 * D:3 * D])
        op("dve", "scalar_tensor_tensor", [msb, nw2], [vec], out=vec[:, 3, :], in0=msb[:, 4 * D:5 * D], scalar=1.0,
           in1=nw2[:, 1, :], op0=ALU.add, op1=ALU.mult)
        op("dve", "tensor_copy", [msb], [vec], out=vec[:, 4, :], in_=msb[:, 3 * D:4 * D])
        op("dve", "tensor_copy", [msb], [vec], out=vec[:, 5, :], in_=msb[:, 5 * D:6 * D])
        fw.dma("sp", mods_d.ap(), vec[:], reads=[vec], writes=[mods_tok])
        with nc.allow_non_contiguous_dma(reason="tiny column-layout load"):
            for r_ in range(2):
                fw.dma("sp", colv[:, r_, :, :], mods_d[r_, :, :].rearrange("v (k p) -> p v k", p=128),
                       reads=[mods_tok], writes=[colv])
        fw.finish([colv, mods_tok])

    def barrier():
        for e in ("pe", "act", "dve", "pool", "sp"):
            for k in ("pe", "act", "dve", "pool"):
                if k != e or e in fw.SAME_ENGINE_SYNC:
                    fw._wait(e, k, fw.seq[k])

    barrier()

    oa_d = nc.dram_tensor("oad", [T, HW], BF16)
    sg_d = nc.dram_tensor("sgd", [T, HW], BF16)
    xacc_tok = fw.token("xacc_tok")
    h2_tok = fw.token("h2_tok")
    ob_tok = fw.token("ob_tok")
    oa_tok = fw.token("oa_tok")
    q_d = nc.dram_tensor("qd", [T, HW], BF16)
    v_d = nc.dram_tensor("vd", [T, HW], BF16)
    q_tok = fw.token("q_tok")
    v_tok = fw.token("v_tok")
    sg_tok = fw.token("sg_tok")

    def V3(buf):
        return buf[:].rearrange("p (h c) -> p h c", h=4)

    def bc4(t):
        return t[:].unsqueeze(2).to_broadcast([128, 4, 128])

    def pipeline(gens, depth=2):
        active = []
        nxt = 0
        while nxt < len(gens) or active:
            if nxt < len(gens) and len(active) < depth and (not active or active[-1][1]):
                active.append([gens[nxt], False])
                nxt += 1
            for a_ in list(active):
                try:
                    m = next(a_[0])
                    if m == "H":
                        a_[1] = True
                except StopIteration:
                    active.remove(a_)

    def proj(lhs, w, kn, c0, ncols=512):
        bk = fw.bank()
        for k in range(kn):
            op("pe", "matmul", [lhs, w], [bk], bk[:, 0:ncols], lhsT=lhs[:, k, :], rhs=w[:, k, c0:c0 + ncols],
               start=(k == 0), stop=(k == kn - 1))
        return bk

    def rstd_from_ss(ss_ap, n, out_ap, rbufs, wbufs, eb=None):
        eb = epsb if eb is None else eb
        op("act", "activation", rbufs + [eb], wbufs, out=out_ap, in_=ss_ap, func=AF.Ln, scale=1.0 / n, bias=eb[:, 0:1])
        op("act", "activation", wbufs, wbufs, out=out_ap, in_=out_ap, func=AF.Exp, scale=-0.5)

    def norm_T(xtile, hb, hT, st, row, rs_ap=None, rs_buf=None, rs_out=None):
        if rs_ap is None:
            op("act", "activation", [xtile], [hb, st], out=hb[:], in_=xtile[:], func=AF.Square, accum_out=st[:, 0:1])
            rstd_from_ss(st[:, 0:1], D, st[:, 1:2], [st], [st])
            if rs_out is not None:
                op("dve", "tensor_copy", [st], [RS], out=rs_out, in_=st[:, 1:2])
            rs_ap, rs_buf = st[:, 1:2], st
        op("dve", "tensor_scalar", [xtile, rs_buf], [hb], out=hb[:], in0=xtile[:], scalar1=rs_ap, scalar2=None,
           op0=ALU.mult)
        bk = fw.bank()
        v = bk[:].bitcast(BF16).rearrange("p (a b) -> p a b", a=8)
        for k in range(KD):
            op("pe", "transpose", [hb, identb], [bk], v[:, k, :], hb[:, k * 128:(k + 1) * 128], identb[:])
        for k in range(KD):
            if k % 2 == 0:
                op("act", "activation", [bk, colv], [hT], out=hT[:, k, :], in_=v[:, k, :], func=AF.Identity,
                   scale=colv[:, row, 0, k:k + 1], bias=colv[:, row, 1, k:k + 1], partial=True)
            else:
                op("dve", "tensor_scalar", [bk, colv], [hT], out=hT[:, k, :], in0=v[:, k, :],
                   scalar1=colv[:, row, 0, k:k + 1], scalar2=colv[:, row, 1, k:k + 1], op0=ALU.mult, op1=ALU.add,
                   partial=True)

    def transpose_to(src, dst, n, eng="act"):
        bk = fw.bank()
        v = bk[:].bitcast(BF16).rearrange("p (a b) -> p a b", a=8)
        for k in range(n):
            op("pe", "transpose", [src, identb], [bk], v[:, k, :], src[:, k * 128:(k + 1) * 128], identb[:])
        if eng == "act":
            op("act", "activation", [bk], [dst], out=dst[:, 0:n, :], in_=v[:, 0:n, :], func=AF.Copy)
        else:
            op("dve", "tensor_copy", [bk], [dst], out=dst[:, 0:n, :], in_=v[:, 0:n, :])

    def silu2_from_bank(bk, tmp, dst):
        op("act", "activation", [bk], [tmp], out=tmp[:], in_=bk[:, :], func=AF.Tanh, scale=0.5)
        op("dve", "scalar_tensor_tensor", [bk, tmp], [dst], out=dst[:], in0=tmp[:], scalar=1.0, in1=bk[:, :],
           op0=ALU.add, op1=ALU.mult)

    def tanh_half_from_bank(bk, dst):
        op("act", "activation", [bk], [dst], out=dst[:], in_=bk[:, :], func=AF.Tanh, scale=0.5)

    hnw = sload("sp", "hnw", [128, HW], F32, hnw_d.ap().to_broadcast([128, HW]), stack=S1)
    snw = sload("sp", "snw", [128, HW], F32, snw_d.ap().to_broadcast([128, HW]), stack=S1)
    sguw = sload("pool", "sguw", [128, 4, 128], BF16, sguw_d.ap(), stack=S1)
    sgub = sload("sp", "sgub", [128, 4], F32, sgub_d.ap(), stack=S1)
    sfinal()
    op("dve", "tensor_scalar", [hnw], [hnw], out=hnw[:], in0=hnw[:], scalar1=0.5, scalar2=None, op0=ALU.mult)
    op("dve", "tensor_scalar", [sguw], [sguw], out=sguw[:], in0=sguw[:], scalar1=0.5, scalar2=None, op0=ALU.mult)
    op("dve", "tensor_scalar", [sgub], [sgub], out=sgub[:], in0=sgub[:], scalar1=0.5, scalar2=None, op0=ALU.mult)
    W = lambda name, shape, dtp, dma=False: fw.sb(name, shape, dtp, dma=dma, stack=S1)
    Sst = [W("S%d" % i, [128, 4, 128], F32) for i in range(2)]
    epsb = W("epsb", [128, 1], F32)
    for s_ in Sst:
        op("pool", "memset", [], [s_], s_[:], 0.0)
    op("pool", "memset", [], [epsb], epsb[:], EPS)
    epsb4 = W("epsb4", [128, 1], F32)
    op("pool", "memset", [], [epsb4], epsb4[:], 4.0 * EPS)
    lnh = W("lnh", [128, 1], F32)
    op("pool", "memset", [], [lnh], lnh[:], -0.6931471805599453)

    class Set:
        pass

    def make_setA(i):
        z = Set()
        n = lambda nm: "%s_%d" % (nm, i)
        z.xt = W(n("xt"), [128, D], F32, dma=True)
        z.obt = W(n("obt"), [128, HW], BF16, dma=True)
        z.st = [W(n("st%d" % j), [128, 8], F32) for j in range(3)]
        z.F = [W(n("F%d" % j), [128, HW], F32) for j in range(6)]
        z.H = [W(n("H%d" % j), [128, HW], BF16, dma=(j in (0, 1, 2))) for j in range(6)]
        z.hb = W(n("hb"), [128, D], BF16)
        z.hT = W(n("hT"), [128, KD, 128], BF16)
        z.QKT = W(n("QKT"), [128, 8, 128], BF16)
        z.bml = W(n("bml"), [128, 4, 2], F32)
        z.ebm = W(n("ebm"), [128, 4], F32)
        z.edl = W(n("edl"), [128, 4], F32)
        z.oast = W(n("oast"), [128, HW], BF16, dma=True)
        z.sgst = W(n("sgst"), [128, HW], BF16, dma=True)
        z.gw = W(n("gw"), [128, HW], BF16)
        z.ug = W(n("ug"), [128, HW], BF16)
        z.vs = W(n("vs"), [128, HW], F32)
        z.gp = W(n("gp"), [128, HW], F32)
        return z

    setsA = [make_setA(i) for i in range(NSA)]

    def hgrn_tile(z, d, want_o):
        sig, ff, logf, kk, Epos, Eneg = z.F
        Sp = z.F[5]
        qs, vv, Qt, Kt, Spb, sT = z.H
        QKT, bml, ebm, edl = z.QKT, z.bml, z.ebm, z.edl
        S = Sst[d]
        Mm = cst[:, (C_MF if d == 0 else C_MB):(C_MF if d == 0 else C_MB) + 128]
        selc2 = cst[:, (C_SELF if d == 0 else C_SELB):(C_SELF if d == 0 else C_SELB) + 2]
        op("pool", "tensor_tensor", [sig, oml], [ff], out=ff[:], in0=sig[:], in1=oml[:, d, :], op=ALU.mult)
        op("pool", "tensor_tensor", [ff, lb], [ff], out=ff[:], in0=ff[:], in1=lb[:, d, :], op=ALU.add)
        op("act", "activation", [ff], [logf], out=logf[:], in_=ff[:], func=AF.Ln)
        op("pool", "tensor_scalar", [ff], [kk], out=kk[:], in0=ff[:], scalar1=-1.0, scalar2=1.0,
           op0=ALU.mult, op1=ALU.add)
        yield
        bb = fw.bank()
        op("pe", "matmul", [cst, logf], [bb], bb[:, :], lhsT=Mm, rhs=logf[:], start=True, stop=True)
        bl = fw.bank()
        blv = bl[:, 0:8].rearrange("p (h c) -> p h c", h=4)
        for h in range(NH):
            op("pe", "matmul", [logf, cst], [bl], blv[:, h, :], lhsT=logf[:, h * 128:(h + 1) * 128], rhs=selc2,
               start=True, stop=True)
        op("act", "activation", [bb], [Eneg], out=Eneg[:], in_=bb[:, :], func=AF.Exp, scale=-1.0)
        if want_o:
            op("act", "activation", [bb], [Epos], out=Epos[:], in_=bb[:, :], func=AF.Exp, bias=lnh[:, 0:1])
        op("dve", "tensor_copy", [bl], [bml], out=bml[:], in_=blv)
        yield
        op("pool", "tensor_tensor", [kk, Eneg], [Kt], out=Kt[:], in0=kk[:], in1=Eneg[:], op=ALU.mult)
        if want_o:
            op("pool", "tensor_tensor", [qs, Epos], [Qt], out=Qt[:], in0=qs[:], in1=Epos[:], op=ALU.mult)
        op("act", "activation", [bml], [ebm], out=ebm[:], in_=bml[:, :, 0], func=AF.Exp)
        op("dve", "tensor_tensor", [bml], [edl], out=edl[:], in0=bml[:, :, 1], in1=bml[:, :, 0], op=ALU.subtract)
        op("act", "activation", [edl], [edl], out=edl[:], in_=edl[:], func=AF.Exp)
        yield "H"
        op("dve", "tensor_tensor", [S, ebm], [Sp], out=V3(Sp), in0=S[:], in1=bc4(ebm), op=ALU.mult)
        z.ob = None
        if want_o:
            op("act", "activation", [Sp], [Spb], out=Spb[:], in_=Sp[:], func=AF.Copy)
            bk = fw.bank()
            v = bk[:].bitcast(BF16).rearrange("p (a b) -> p a b", a=8)
            for h in range(NH):
                op("pe", "transpose", [Qt, identb], [bk], v[:, h, :], Qt[:, h * 128:(h + 1) * 128], identb[:])
            for h in range(NH):
                op("pe", "transpose", [Kt, identb], [bk], v[:, 4 + h, :], Kt[:, h * 128:(h + 1) * 128], identb[:])
            op("act", "activation", [bk], [QKT], out=QKT[:], in_=v, func=AF.Copy)
            yield
            sc = fw.bank()
            scv = sc[:, :].rearrange("p (h c) -> p h c", h=4)
            for h in range(NH):
                if d == 0:
                    op("pe", "matmul", [QKT], [sc], scv[0:64, h, :], lhsT=QKT[:, 4 + h, 0:64], rhs=QKT[:, h, :],
                       start=True, stop=True)
                    op("pe", "matmul", [QKT], [sc], scv[64:128, h, 64:128], lhsT=QKT[:, 4 + h, 64:128],
                       rhs=QKT[:, h, 64:128], start=True, stop=True)
                else:
                    op("pe", "matmul", [QKT], [sc], scv[0:64, h, 0:64], lhsT=QKT[:, 4 + h, 0:64],
                       rhs=QKT[:, h, 0:64], start=True, stop=True)
                    op("pe", "matmul", [QKT], [sc], scv[64:128, h, :], lhsT=QKT[:, 4 + h, 64:128], rhs=QKT[:, h, :],
                       start=True, stop=True)
            op("pool", "memset", [], [sT], sT[:], 0.0)
            op("dve", "copy_predicated", [sc, maski], [sT], V3(sT),
               maski[:, d, :].unsqueeze(1).to_broadcast([128, 4, 128]), scv)
            yield
        ub = fw.bank()
        for h in range(NH):
            hs = slice(h * 128, (h + 1) * 128)
            op("pe", "matmul", [Kt, vv], [ub], ub[:, hs], lhsT=Kt[:, hs], rhs=vv[:, hs], start=True, stop=True)
        op("dve", "tensor_tensor", [ub, Sp], [Sp], out=Sp[:], in0=ub[:, :], in1=Sp[:], op=ALU.add)
        if want_o:
            ob = fw.bank()
            obv = ob[:, :].rearrange("p (h c) -> p h c", h=4)
            for h in range(NH):
                hs = slice(h * 128, (h + 1) * 128)
                op("pe", "matmul", [sT, vv], [ob], obv[:, h, :], lhsT=sT[:, hs], rhs=vv[:, hs],
                   start=True, stop=False)
                op("pe", "matmul", [QKT, Spb], [ob], obv[:, h, :], lhsT=QKT[:, h, :], rhs=Spb[:, hs],
                   start=False, stop=True)
            z.ob = ob
        op("dve", "tensor_tensor", [Sp, edl], [S], out=S[:], in0=V3(Sp), in1=bc4(edl), op=ALU.mult)

    def gelu2_from_bank(z, bk, dst):
        gp = z.gp
        op("act", "activation", [bk], [gp], out=gp[:], in_=bk[:, :], func=AF.Square, scale=0.044715 ** 0.5)
        op("dve", "scalar_tensor_tensor", [gp, bk], [gp], out=gp[:], in0=gp[:], scalar=1.0, in1=bk[:, :],
           op0=ALU.add, op1=ALU.mult)
        op("act", "activation", [gp], [gp], out=gp[:], in_=gp[:], func=AF.Tanh, scale=0.5 * GELU_C)
        op("dve", "scalar_tensor_tensor", [gp, bk], [dst], out=dst[:], in0=gp[:], scalar=1.0, in1=bk[:, :],
           op0=ALU.add, op1=ALU.mult)

    def group_rms(z, buf, rs_tile, eb=None):
        sq = z.F[0]
        op("pool", "tensor_tensor", [buf], [sq], out=sq[:], in0=buf[:], in1=buf[:], op=ALU.mult)
        op("dve", "tensor_reduce", [sq], [rs_tile], out=rs_tile[:, 4:8], in_=V3(sq), axis=AX.X, op=ALU.add)
        rstd_from_ss(rs_tile[:, 4:8], 128, rs_tile[:, 0:4], [rs_tile], [rs_tile], eb)

    def body_ctx(z, ti, d):
        fw.dma("sp", z.xt[:], ctx_d[ti * 128:(ti + 1) * 128, :], writes=[z.xt])
        norm_T(z.xt, z.hb, z.hT, z.st[0], 1)
        bz = proj(z.hT, win, KD, 512 * (1 + d))
        tanh_half_from_bank(bz, z.F[0])
        bv = proj(z.hT, win, KD, 512 * 3)
        op("dve", "tensor_copy", [bv], [z.H[1]], out=z.H[1][:], in_=bv[:, :])
        for _ in hgrn_tile(z, d, False):
            pass
        yield "H"

    def body_p1(z, ti):
        fw.dma("sp", z.xt[:], x_d[ti * 128:(ti + 1) * 128, :], writes=[z.xt])
        norm_T(z.xt, z.hb, z.hT, z.st[0], 0, rs_out=RS[:, ti:ti + 1])
        yield
        bq = proj(z.hT, win, KD, 0)
        silu2_from_bank(bq, z.F[0], z.H[0])
        fw.dma("sp", q_d[ti * 128:(ti + 1) * 128, :], z.H[0][:], reads=[z.H[0]], writes=[q_tok])
        yield
        bz = proj(z.hT, win, KD, 512 * 2)
        tanh_half_from_bank(bz, z.F[0])
        bv = proj(z.hT, win, KD, 512 * 3)
        op("dve", "tensor_copy", [bv], [z.H[1]], out=z.H[1][:], in_=bv[:, :])
        fw.dma("sp", v_d[ti * 128:(ti + 1) * 128, :], z.H[1][:], reads=[z.H[1]], writes=[v_tok])
        yield
        for m_ in hgrn_tile(z, 1, True):
            yield m_
        obf = z.H[2]
        op("act", "activation", [z.ob], [obf], out=obf[:], in_=z.ob[:, :], func=AF.Copy)
        fw.dma("sp", ob_d[ti * 128:(ti + 1) * 128, :], obf[:], reads=[obf], writes=[ob_tok])
        yield

    def body_p2(z, ti):
        fw.dma("act", z.obt[:], ob_d[ti * 128:(ti + 1) * 128, :], reads=[ob_tok], writes=[z.obt])
        fw.dma("sp", z.xt[:], x_d[ti * 128:(ti + 1) * 128, :], writes=[z.xt])
        norm_T(z.xt, z.hb, z.hT, z.st[0], 0, rs_ap=RS[:, ti:ti + 1], rs_buf=RS)
        yield
        fw.dma("act", z.H[0][:], q_d[ti * 128:(ti + 1) * 128, :], reads=[q_tok], writes=[z.H[0]])
        fw.dma("act", z.H[1][:], v_d[ti * 128:(ti + 1) * 128, :], reads=[v_tok], writes=[z.H[1]])
        bg = proj(z.hT, win, KD, 512 * 4)
        silu2_from_bank(bg, z.gp, z.vs)
        op("pool", "tensor_tensor", [z.vs, hnw], [z.gw], out=z.gw[:], in0=z.vs[:], in1=hnw[:], op=ALU.mult)
        yield
        bu = proj(z.hT, win, KD, 512 * 5)
        gelu2_from_bank(z, bu, z.ug)
        yield
        bs = proj(z.hT, win, KD, 512 * 6)
        gelu2_from_bank(z, bs, z.vs)
        yield
        bz = proj(z.hT, win, KD, 512 * 1)
        tanh_half_from_bank(bz, z.F[0])
        yield
        for m_ in hgrn_tile(z, 0, True):
            yield m_
        osum = z.F[4]
        op("dve", "tensor_tensor", [z.ob, z.obt], [osum], out=osum[:], in0=z.ob[:, :], in1=z.obt[:], op=ALU.add)
        yield
        group_rms(z, osum, z.st[1])
        for h in range(NH):
            hs = slice(h * 128, (h + 1) * 128)
            op("dve", "scalar_tensor_tensor", [osum, z.st[1], z.gw], [z.oast], out=z.oast[:, hs], in0=osum[:, hs],
               scalar=z.st[1][:, h:h + 1], in1=z.gw[:, hs], op0=ALU.mult, op1=ALU.mult, same_slice=(h > 0))
        fw.dma("sp", oa_d[ti * 128:(ti + 1) * 128, :], z.oast[:], reads=[z.oast], writes=[oa_tok])
        yield
        vs, vn, ug = z.vs, z.H[5], z.ug
        group_rms(z, vs, z.st[2], epsb4)
        for g in range(4):
            hs = slice(g * 128, (g + 1) * 128)
            op("dve", "scalar_tensor_tensor", [vs, z.st[2], snw], [vn], out=vn[:, hs], in0=vs[:, hs],
               scalar=z.st[2][:, g:g + 1], in1=snw[:, hs], op0=ALU.mult, op1=ALU.mult, same_slice=(g > 0))
        yield
        mb_ = fw.bank()
        for g in range(4):
            hs = slice(g * 128, (g + 1) * 128)
            op("pe", "matmul", [sguw, vn], [mb_], mb_[:, hs], lhsT=sguw[:, g, :], rhs=vn[:, hs], start=True, stop=True)
        for g in range(4):
            hs = slice(g * 128, (g + 1) * 128)
            op("dve", "scalar_tensor_tensor", [mb_, sgub, ug], [z.sgst], out=z.sgst[:, hs], in0=mb_[:, hs],
               scalar=sgub[:, g:g + 1], in1=ug[:, hs], op0=ALU.add, op1=ALU.mult, same_slice=(g > 0))
        fw.dma("sp", sg_d[ti * 128:(ti + 1) * 128, :], z.sgst[:], reads=[z.sgst], writes=[sg_tok])
        yield

    gens = []
    for d in (0, 1):
        for ti in ((0, 1) if d == 0 else (1, 0)):
            gens.append(body_ctx(setsA[len(gens) % 2], ti, d))
    fw.begin_defer()
    pipeline(gens)
    fw.flush(WIN)
    fw.begin_defer()
    pipeline([body_p1(setsA[i % NSA], ti) for i, ti in enumerate(reversed(range(NT)))], NSA)
    fw.flush(WIN)
    fw.begin_defer()
    pipeline([body_p2(setsA[ti % NSA], ti) for ti in range(NT)], NSA)

    fw.flush(WIN)
    barrier()
    S1.close()

    S1 = ExitStack()
    W = lambda name, shape, dtp, dma=False: fw.sb(name, shape, dtp, dma=dma, stack=S1)
    rw = sload("sp", "rw", [128, KD, NE], F32, rw_d.ap().rearrange("(k p) e -> p k e", p=128), stack=S1)
    Gm = sload("sp", "Gm", [128, D], F32, mods_d[0, 2:3, :].to_broadcast([128, D]), stack=S1)
    Af = sload("sp", "Af", [128, D], F32, mods_d[0, 3:4, :].to_broadcast([128, D]), stack=S1)
    Bf = sload("sp", "Bf", [128, D], F32, mods_d[0, 4:5, :].to_broadcast([128, D]), stack=S1)
    sfinal()
    op("dve", "tensor_scalar", [Gm], [Gm], out=Gm[:], in0=Gm[:], scalar1=0.5, scalar2=None, op0=ALU.mult)
    winB = W("winB", [128, KD, 2048], BF16, dma=True)
    for k in range(KD):
        fw.dma("pool", winB[:, k, :], win_d[k * 128:(k + 1) * 128, NA:INC], writes=[winB])
    wa = W("wa", [128, 4, D], BF16, dma=True)
    fw.dma("pool", wa[:], wa_d.ap().rearrange("(k p) n -> p k n", p=128), writes=[wa])
    wb = W("wb", [128, 4, D], BF16, dma=True)
    fw.dma("pool", wb[:], wb_d.ap().rearrange("(k p) n -> p k n", p=128), writes=[wb])
    wo = W("wo", [128, KD, D], BF16, dma=True)
    fw.dma("pool", wo[:], wo_d.ap().rearrange("(k p) n -> p k n", p=128), writes=[wo])
    epsb = W("epsb3", [128, 1], F32)
    op("pool", "memset", [], [epsb], epsb[:], EPS)

    def make_setC(i):
        z = Set()
        n = lambda nm: "%s_c%d" % (nm, i)
        z.xt = W(n("xt"), [128, D], F32, dma=True)
        z.oat = W(n("oat"), [128, HW], BF16, dma=True)
        z.sgt = W(n("sgt"), [128, HW], BF16, dma=True)
        z.st = [W(n("st%d" % j), [128, 8], F32) for j in range(2)]
        z.big = W(n("big"), [128, D], BF16, dma=True)
        z.hT = W(n("hT"), [128, KD, 128], BF16)
        z.tT = W(n("tT"), [128, KD, 128], BF16)
        z.sgab = W(n("sgab"), [128, D], BF16)
        z.m1 = W(n("m1"), [128, D], F32)
        z.tmp = [W(n("tmp%d" % j), [128, HW], F32) for j in range(2)]
        z.lg = W(n("lg"), [128, NE], F32)
        return z

    setsC = [make_setC(i) for i in range(NSC)]

    def body_p3(z, ti):
        xtile, m1, sgab, big = z.xt, z.m1, z.sgab, z.big
        fw.dma("act", z.oat[:], oa_d[ti * 128:(ti + 1) * 128, :], reads=[oa_tok], writes=[z.oat])
        fw.dma("act", z.sgt[:], sg_d[ti * 128:(ti + 1) * 128, :], reads=[sg_tok], writes=[z.sgt])
        fw.dma("sp", xtile[:], x_d[ti * 128:(ti + 1) * 128, :], writes=[xtile])
        norm_T(xtile, big, z.hT, z.st[0], 0, rs_ap=RS[:, ti:ti + 1], rs_buf=RS)
        yield
        transpose_to(z.oat, z.tT, 4, eng="dve")
        ya = [proj(z.tT, wa, 4, 0), proj(z.tT, wa, 4, 512)]
        for hh in range(2):
            bga = proj(z.hT, winB, KD, hh * 512)
            op("act", "activation", [bga], [sgab], out=sgab[:, hh * 512:(hh + 1) * 512], in_=bga[:, :], func=AF.Tanh,
               scale=0.5)
        for hh in range(2):
            cs = slice(hh * 512, (hh + 1) * 512)
            op("dve", "scalar_tensor_tensor", [ya[hh], sgab], [m1], out=m1[:, cs], in0=sgab[:, cs], scalar=1.0,
               in1=ya[hh][:, :], op0=ALU.add, op1=ALU.mult)
        yield
        transpose_to(z.sgt, z.tT, 4, eng="dve")
        yb = [proj(z.tT, wb, 4, 0), proj(z.tT, wb, 4, 512)]
        for hh in range(2):
            bgb = proj(z.hT, winB, KD, 1024 + hh * 512)
            op("act", "activation", [bgb], [sgab], out=sgab[:, hh * 512:(hh + 1) * 512], in_=bgb[:, :], func=AF.Tanh,
               scale=0.5)
        for hh in range(2):
            cs = slice(hh * 512, (hh + 1) * 512)
            tmp = z.tmp[hh]
            op("dve", "scalar_tensor_tensor", [yb[hh], sgab], [tmp], out=tmp[:], in0=sgab[:, cs], scalar=1.0,
               in1=yb[hh][:, :], op0=ALU.add, op1=ALU.mult)
            op("dve", "tensor_tensor", [tmp, m1], [big], out=big[:, cs], in0=tmp[:], in1=m1[:, cs], op=ALU.add)
        yield
        transpose_to(big, z.tT, 8, eng="act")
        yo = [proj(z.tT, wo, KD, 0), proj(z.tT, wo, KD, 512)]
        for hh in range(2):
            cs = slice(hh * 512, (hh + 1) * 512)
            op("dve", "tensor_tensor", [yo[hh], Gm], [m1], out=m1[:, cs], in0=yo[hh][:, :], in1=Gm[:, cs], op=ALU.mult)
        op("dve", "tensor_tensor", [m1, xtile], [xtile], out=xtile[:], in0=m1[:], in1=xtile[:], op=ALU.add)
        fw.dma("sp", xacc_d[ti * 128:(ti + 1) * 128, :], xtile[:], reads=[xtile], writes=[xacc_tok])
        yield
        st = z.st[1]
        op("act", "activation", [xtile], [big, st], out=big[:], in_=xtile[:], func=AF.Square, accum_out=st[:, 0:1])
        rstd_from_ss(st[:, 0:1], D, st[:, 1:2], [st], [st])
        op("dve", "scalar_tensor_tensor", [xtile, st, Af], [m1], out=m1[:], in0=xtile[:], scalar=st[:, 1:2], in1=Af[:],
           op0=ALU.mult, op1=ALU.mult)
        op("dve", "tensor_tensor", [m1, Bf], [big], out=big[:], in0=m1[:], in1=Bf[:], op=ALU.add)
        fw.dma("sp", h2_d[ti * 128:(ti + 1) * 128, :], big[:], reads=[big], writes=[h2_tok])
        yield
        bks = [fw.bank(), fw.bank()]
        for k in range(KD):
            v = bks[k // 4][:, :].rearrange("p (a b) -> p a b", a=4)
            op("pe", "transpose", [m1, cst], [bks[k // 4]], v[:, k % 4, :], m1[:, k * 128:(k + 1) * 128], ident)
        h2T = m1[:].rearrange("p (k t) -> p k t", k=KD)
        for k in range(KD):
            v = bks[k // 4][:, :].rearrange("p (a b) -> p a b", a=4)
            op("act", "activation", [bks[k // 4], colv], [m1], out=h2T[:, k, :], in_=v[:, k % 4, :], func=AF.Identity,
               bias=colv[:, 0, 4, k:k + 1])
        bl_ = fw.bank()
        for k in range(KD):
            op("pe", "matmul", [m1, rw], [bl_], bl_[:, 0:NE], lhsT=h2T[:, k, :], rhs=rw[:, k, :],
               start=(k == 0), stop=(k == KD - 1))
        op("dve", "tensor_reduce", [bl_], [st], out=st[:, 2:3], in_=bl_[:, 0:NE], axis=AX.X, op=ALU.max)
        op("dve", "tensor_scalar", [st], [st], out=st[:, 3:4], in0=st[:, 2:3], scalar1=-1.0, scalar2=None, op0=ALU.mult)
        op("act", "activation", [bl_, st], [z.lg, st], out=z.lg[:], in_=bl_[:, 0:NE], func=AF.Exp, bias=st[:, 3:4],
           accum_out=st[:, 4:5])
        op("dve", "reciprocal", [st], [st], out=st[:, 5:6], in_=st[:, 4:5])
        op("dve", "tensor_scalar", [z.lg, st], [AFF], out=AFF[:, ti, :], in0=z.lg[:], scalar1=st[:, 5:6], scalar2=None,
           op0=ALU.mult)
        yield "H"

    def p3_wrap(z, ti):
        first = True
        for m in body_p3(z, ti):
            yield "H"

    fw.begin_defer()
    pipeline([p3_wrap(setsC[ti % NSC], ti) for ti in range(NT)], NSC)
    fw.flush(WIN)

    if debug == "xmid":
        fw.dma("sp", aff_dbg.ap(), AFF[:], reads=[AFF], writes=[xacc_tok], sem=fw.new_dsem("affdbg"))
        fw.finish([xacc_tok, h2_tok])
        print("ins", fw.n_ins, "waits", fw.n_wait, fw.seq)
        return nc

    barrier()
    S1.close()

    S2 = ExitStack()
    M = lambda name, shape, dtp, dma=False, stack=S2: fw.sb(name, shape, dtp, dma=dma, stack=stack)
    wgp = [M("wg%d" % i, [128, KD, 512], BF16, dma=True) for i in range(4)]
    wup = [M("wu%d" % i, [128, KD, 512], BF16, dma=True) for i in range(4)]
    wdp = [M("wd%d" % i, [128, 16, 512], BF16, dma=True) for i in range(2)]
    idx_all = M("idx_all", [128, NE, SB], I32, dma=True)
    gate_all = M("gate_all", [128, NE, SB], F32)
    gf_bc = M("gf_bc", [128, D], F32, dma=True)
    fw.dma("sp", gf_bc[:], mods_d[0, 5:6, :].to_broadcast([128, D]), reads=[mods_tok], writes=[gf_bc])

    def load_expert(e, parts=(0, 1, 2, 3)):
        for i in parts:
            fw.dma("pool", wgp[i][:], wg_l[e][:, i * 512:(i + 1) * 512].rearrange("(k p) n -> p k n", p=128),
                   writes=[wgp[i]])
            fw.dma("pool", wup[i][:], wu_l[e][:, i * 512:(i + 1) * 512].rearrange("(k p) n -> p k n", p=128),
                   writes=[wup[i]])

    def load_expert_down(e):
        for i in range(2):
            fw.dma("pool", wdp[i][:], wd_l[e][:, i * 512:(i + 1) * 512].rearrange("(k p) n -> p k n", p=128),
                   writes=[wdp[i]])

    if debug != "route":
        load_expert(0)
        load_expert_down(0)

    with ExitStack() as SR:
        R = lambda name, shape, dtp: fw.sb(name, shape, dtp, stack=SR)
        NTE = NT * NE
        cst = fw.sb("cstB", [128, NCST], F32, dma=True, stack=SR)
        fw.dma("sp", cst[:], cst_d.ap(), writes=[cst])
        ones = cst[:, C_ONES:C_ONES + 128]
        lo = R("lo", [128, NE], F32)
        hi = R("hi", [128, NE], F32)
        mid = R("mid", [128, NE], F32)
        cnt = R("cnt", [128, NE], F32)
        ge = R("ge", [128, NE], F32)
        dd = R("dd", [128, NE], F32)
        cmp_ = R("cmp", [128, NT, NE], F32)
        op("dve", "memset", [], [lo], lo[:], 0.0)
        op("dve", "memset", [], [hi], hi[:], 1.0)
        for it in range(n_bisect):
            op("dve", "tensor_tensor", [lo, hi], [mid], out=mid[:], in0=lo[:], in1=hi[:], op=ALU.add)
            op("dve", "tensor_scalar", [mid], [mid], out=mid[:], in0=mid[:], scalar1=0.5, scalar2=None, op0=ALU.mult)
            op("dve", "tensor_tensor", [AFF, mid], [cmp_], out=cmp_[:], in0=AFF[:],
               in1=mid[:].unsqueeze(1).to_broadcast([128, NT, NE]), op=ALU.is_ge)
            op("dve", "tensor_reduce", [cmp_], [cnt], out=cnt[:], in_=cmp_[:].rearrange("p j e -> p e j"), axis=AX.X,
               op=ALU.add)
            bk = fw.bank()
            op("pe", "matmul", [cst, cnt], [bk], bk[:, 0:NE], lhsT=ones, rhs=cnt[:], start=True, stop=True)
            op("dve", "tensor_scalar", [bk], [ge], out=ge[:], in0=bk[:, 0:NE], scalar1=float(CAP) - 0.5, scalar2=None,
               op0=ALU.is_ge)
            op("dve", "tensor_tensor", [mid, ge], [dd], out=dd[:], in0=mid[:], in1=ge[:], op=ALU.mult)
            op("dve", "tensor_tensor", [lo, dd], [lo], out=lo[:], in0=lo[:], in1=dd[:], op=ALU.max)
            op("dve", "tensor_tensor", [mid, ge], [dd], out=dd[:], in0=mid[:], in1=ge[:], op=ALU.add)
            op("dve", "tensor_tensor", [hi, dd], [hi], out=hi[:], in0=hi[:], in1=dd[:], op=ALU.min)
        sel = R("sel", [128, NT, NE], F32)
        op("dve", "tensor_tensor", [AFF, lo], [sel], out=sel[:], in0=AFF[:],
           in1=lo[:].unsqueeze(1).to_broadcast([128, NT, NE]), op=ALU.is_ge)
        self2 = sel[:].rearrange("p j e -> p (j e)")
        pos = R("pos", [128, NT, NE], F32)
        pos2 = pos[:].rearrange("p j e -> p (j e)")
        tot = R("tot", [128, NT, NE], F32)
        tot2 = tot[:].rearrange("p j e -> p (j e)")
        cA = R("cA", [128, NT, NE], F32)
        cB = R("cB", [128, NT, NE], F32)
        lstr = cst[:, C_LSTR:C_LSTR + 128]
        ncol = (NTE + 511) // 512
        for c in range(ncol):
            c0, c1 = c * 512, min(NTE, (c + 1) * 512)
            bk = fw.bank()
            op("pe", "matmul", [cst, sel], [bk], bk[:, 0:c1 - c0], lhsT=lstr, rhs=self2[:, c0:c1], start=True, stop=True)
            op("dve", "tensor_copy", [bk], [pos], out=pos2[:, c0:c1], in_=bk[:, 0:c1 - c0])
            bk = fw.bank()
            op("pe", "matmul", [cst, sel], [bk], bk[:, 0:c1 - c0], lhsT=ones, rhs=self2[:, c0:c1], start=True, stop=True)
            op("dve", "tensor_copy", [bk], [tot], out=tot2[:, c0:c1], in_=bk[:, 0:c1 - c0])
        src = tot
        op("dve", "tensor_copy", [tot], [cA], out=cA[:], in_=tot[:])
        cur, nxt = cA, cB
        s_ = 1
        while s_ < NT:
            op("dve", "tensor_tensor", [cur], [nxt], out=nxt[:, s_:, :], in0=cur[:, s_:, :], in1=cur[:, :NT - s_, :],
               op=ALU.add)
            op("dve", "tensor_copy", [cur], [nxt], out=nxt[:, :s_, :], in_=cur[:, :s_, :])
            cur, nxt = nxt, cur
            s_ *= 2
        op("dve", "tensor_tensor", [cur, tot], [cur], out=cur[:], in0=cur[:], in1=tot[:], op=ALU.subtract)
        op("dve", "tensor_tensor", [pos, cur], [pos], out=pos[:], in0=pos[:], in1=cur[:], op=ALU.add)
        op("dve", "scalar_tensor_tensor", [pos, sel], [pos], out=pos2, in0=pos2, scalar=1.0, in1=self2,
           op0=ALU.add, op1=ALU.mult)
        op("dve", "tensor_scalar", [pos], [pos], out=pos[:], in0=pos[:], scalar1=-1.0, scalar2=None, op0=ALU.add)
        pdiv = nxt
        op("dve", "memset", [], [pdiv], pdiv[:], 0.0)
        pdiv2 = pdiv[:].rearrange("p j e -> p (j e)")
        for b_ in range(1, SB):
            op("dve", "scalar_tensor_tensor", [pos, pdiv], [pdiv], out=pdiv2, in0=pos2, scalar=128.0 * b_ - 0.5,
               in1=pdiv2, op0=ALU.is_ge, op1=ALU.add)
        pmod = cur
        pmod2 = pmod[:].rearrange("p j e -> p (j e)")
        op("dve", "scalar_tensor_tensor", [pdiv, pos], [pmod], out=pmod2, in0=pdiv2, scalar=-128.0, in1=pos2,
           op0=ALU.mult, op1=ALU.add)
        ahi = R("ahi", [128, NT, NE], BF16)
        alo = R("alo", [128, NT, NE], BF16)
        op("dve", "tensor_copy", [AFF], [ahi], out=ahi[:], in_=AFF[:])
        op("dve", "tensor_tensor", [AFF, ahi], [alo], out=alo[:], in0=AFF[:], in1=ahi[:], op=ALU.subtract)
        Be = R("Be", [128, NT, 128], BF16)
        Ae = R("Ae", [128, NT, 8], F32)
        Re = R("Re", [128, NT, 4, 8], BF16)
        iosi = cst[:, C_IOSI:C_IOSI + 128]
        iosb = cst[:, C_IOSB:C_IOSB + 8]
        jv = cst[:, C_JV:C_JV + NT]
        tokf = R("tokf", [128, SB], F32)
        rsb = R("rsb", [128, 32], F32)
        for e in range(NE):
            for j in range(NT):
                op("dve", "tensor_scalar", [cst, pmod], [Be], out=Be[:, j, :], in0=iosi, scalar1=pmod[:, j, e:e + 1],
                   scalar2=None, op0=ALU.is_equal, same_slice=(j > 0))
            op("dve", "tensor_tensor", [cst, pdiv], [Ae], out=Ae[:], in0=iosb.unsqueeze(1).to_broadcast([128, NT, 8]),
               in1=pdiv[:, :, e:e + 1].to_broadcast([128, NT, 8]), op=ALU.is_equal)
            op("dve", "tensor_scalar", [Ae, cst], [Re], out=Re[:, :, 0, :], in0=Ae[:], scalar1=cst[:, C_PV:C_PV + 1],
               scalar2=None, op0=ALU.mult)
            op("dve", "tensor_tensor", [Ae, cst], [Re], out=Re[:, :, 1, :], in0=Ae[:],
               in1=jv.unsqueeze(2).to_broadcast([128, NT, 8]), op=ALU.mult)
            op("dve", "tensor_tensor", [Ae, ahi], [Re], out=Re[:, :, 2, :], in0=Ae[:],
               in1=ahi[:, :, e:e + 1].to_broadcast([128, NT, 8]), op=ALU.mult)
            op("dve", "tensor_tensor", [Ae, alo], [Re], out=Re[:, :, 3, :], in0=Ae[:],
               in1=alo[:, :, e:e + 1].to_broadcast([128, NT, 8]), op=ALU.mult)
            bk = fw.bank()
            for j in range(NT):
                op("pe", "matmul", [Be, Re], [bk], bk[:, 0:32], lhsT=Be[:, j, :],
                   rhs=Re[:, j, :, :].rearrange("p c s -> p (c s)"), start=(j == 0), stop=(j == NT - 1))
            op("dve", "tensor_copy", [bk], [rsb], out=rsb[:], in_=bk[:, 0:32])
            op("dve", "scalar_tensor_tensor", [rsb], [tokf], out=tokf[:], in0=rsb[:, 8:8 + SB], scalar=128.0,
               in1=rsb[:, 0:SB], op0=ALU.mult, op1=ALU.add)
            op("dve", "tensor_scalar", [tokf], [tokf], out=tokf[:], in0=tokf[:], scalar1=-1.0, scalar2=None, op0=ALU.add)
            op("dve", "tensor_copy", [tokf], [idx_all], out=idx_all[:, e, :], in_=tokf[:])
            op("dve", "tensor_tensor", [rsb], [gate_all], out=gate_all[:, e, :], in0=rsb[:, 16:16 + SB],
               in1=rsb[:, 24:24 + SB], op=ALU.add)
        if debug == "route":
            dbg_tok = fw.token("dbg")
            fw.dma("sp", idx_dbg.ap(), idx_all[:], reads=[idx_all], writes=[dbg_tok], sem=idx_all.dsem)
            fw.dma("sp", aff_dbg.ap(), AFF[:], reads=[AFF], writes=[dbg_tok], sem=fw.new_dsem("affdbg"))
            gsem = fw.new_dsem("gdbg")
            fw.dma("sp", gate_dbg.ap(), gate_all[:], reads=[gate_all], writes=[dbg_tok], sem=gsem)
            fw.finish([dbg_tok, xacc_tok, h2_tok])
            print("ins", fw.n_ins, "waits", fw.n_wait, fw.seq)
            return nc
        barrier()

    xg = [M("xg%d" % i, [128, D], BF16, dma=True) for i in range(4)]
    xsT2 = [M("xsT%d" % i, [128, KD, CAP], BF16) for i in range(2)]
    hidT = M("hidT", [128, 16, CAP], BF16)
    sgt = M("sgt", [128, 512], F32)
    yo_ = [M("yo%d" % i, [128, D], F32, dma=True) for i in range(2)]
    scat_tok = [fw.token("scat0"), fw.token("scat1")]
    NS = (CAP + 511) // 512
    GA = list(range((SB + 1) // 2))
    GB = list(range((SB + 1) // 2, SB))

    def issue_gathers(e, sbs):
        for sb_ in sbs:
            g_ = xg[sb_ % 4]
            fw.dma("pool", g_[:], h2_d[:, :], reads=[h2_tok, idx_all], writes=[g_],
                   indirect=dict(out_offset=None,
                                 in_offset=bass.IndirectOffsetOnAxis(ap=idx_all[:, e, sb_:sb_ + 1], axis=0)))

    def do_transposes(e, sbs):
        xs = xsT2[e % 2]
        for sb_ in sbs:
            g_ = xg[sb_ % 4]
            bk = fw.bank()
            v = bk[:].bitcast(BF16).rearrange("p (a b) -> p a b", a=8)
            for k in range(KD):
                op("pe", "transpose", [g_, identb], [bk], v[:, k, :], g_[:, k * 128:(k + 1) * 128], identb[:])
            op("act", "activation", [bk], [xs], out=xs[:, :, sb_ * 128:(sb_ + 1) * 128], in_=v, func=AF.Copy)

    issue_gathers(0, GA)
    do_transposes(0, GA)
    issue_gathers(0, GB)
    do_transposes(0, GB)
    for e in range(NE):
        xsT = xsT2[e % 2]
        more = e + 1 < NE
        for fq in range(4):
            for fc in range(4):
                f_ = fq * 4 + fc
                for s in range(NS):
                    s0, s1 = s * 512, min(CAP, (s + 1) * 512)
                    bg_ = fw.bank()
                    for k in range(KD):
                        op("pe", "matmul", [wgp[fq], xsT], [bg_], bg_[:, 0:s1 - s0],
                           lhsT=wgp[fq][:, k, fc * 128:(fc + 1) * 128], rhs=xsT[:, k, s0:s1],
                           start=(k == 0), stop=(k == KD - 1))
                    bu_ = fw.bank()
                    for k in range(KD):
                        op("pe", "matmul", [wup[fq], xsT], [bu_], bu_[:, 0:s1 - s0],
                           lhsT=wup[fq][:, k, fc * 128:(fc + 1) * 128], rhs=xsT[:, k, s0:s1],
                           start=(k == 0), stop=(k == KD - 1))
                    op("act", "activation", [bg_], [sgt], out=sgt[:, 0:s1 - s0], in_=bg_[:, 0:s1 - s0], func=AF.Sigmoid)
                    op("dve", "tensor_tensor", [bg_, sgt], [sgt], out=sgt[:, 0:s1 - s0], in0=bg_[:, 0:s1 - s0],
                       in1=sgt[:, 0:s1 - s0], op=ALU.mult)
                    op("dve", "tensor_tensor", [bu_, sgt], [hidT], out=hidT[:, f_, s0:s1], in0=bu_[:, 0:s1 - s0],
                       in1=sgt[:, 0:s1 - s0], op=ALU.mult)
            if more:
                load_expert(e + 1, (fq,))
        if more:
            issue_gathers(e + 1, GA)
        for sb_ in range(SB):
            if more and sb_ == SB // 2:
                do_transposes(e + 1, GA)
                issue_gathers(e + 1, GB)
            y_ = yo_[sb_ % 2]
            for hh in range(2):
                bd = fw.bank()
                for f_ in range(16):
                    op("pe", "matmul", [hidT, wdp[hh]], [bd], bd[:, :], lhsT=hidT[:, f_, sb_ * 128:(sb_ + 1) * 128],
                       rhs=wdp[hh][:, f_, :], start=(f_ == 0), stop=(f_ == 15))
                op("dve", "scalar_tensor_tensor", [bd, gate_all, gf_bc], [y_], out=y_[:, hh * 512:(hh + 1) * 512],
                   in0=bd[:, :], scalar=gate_all[:, e, sb_:sb_ + 1], in1=gf_bc[:, hh * 512:(hh + 1) * 512],
                   op0=ALU.mult, op1=ALU.mult)
            fw.dma("pool", xacc_d[:, :], y_[:], reads=[y_, idx_all], writes=[scat_tok[e % 2]],
                   extra=[scat_tok[(e + 1) % 2], xacc_tok], nowait_w=True,
                   indirect=dict(out_offset=bass.IndirectOffsetOnAxis(ap=idx_all[:, e, sb_:sb_ + 1], axis=0),
                                 in_offset=None, compute_op=ALU.add))
        if more:
            load_expert_down(e + 1)
            do_transposes(e + 1, GB)

    barrier()
    S2.close()
    S3 = ExitStack()
    xf = [fw.sb("xf%d" % i, [128, D], F32, dma=True, stack=S3) for i in range(4)]
    of = [fw.sb("of%d" % i, [128, D], F32, dma=True, stack=S3) for i in range(4)]
    jk = fw.sb("jk", [128, D], BF16, stack=S3)
    stf = fw.sb("stf", [128, 4], F32, stack=S3)
    eps2 = fw.sb("eps2", [128, 1], F32, stack=S3)
    finw = fw.sb("finw", [128, D], F32, dma=True, stack=S3)
    fw.dma("sp", finw[:], nw_d[2:3, :].to_broadcast([128, D]), writes=[finw])
    op("pool", "memset", [], [eps2], eps2[:], EPS)
    out_tok = fw.token("out_tok")
    for ti in range(NT):
        x_ = xf[ti % 4]
        o_ = of[ti % 4]
        fw.dma("sp" if ti % 2 == 0 else "pool", x_[:], xacc_d[ti * 128:(ti + 1) * 128, :], reads=[xacc_tok, scat_tok[0], scat_tok[1]], writes=[x_])
        op("act", "activation", [x_], [jk, stf], out=jk[:], in_=x_[:], func=AF.Square, accum_out=stf[:, 0:1])
        op("act", "activation", [stf, eps2], [stf], out=stf[:, 1:2], in_=stf[:, 0:1], func=AF.Ln, scale=1.0 / D,
           bias=eps2[:, 0:1])
        op("act", "activation", [stf], [stf], out=stf[:, 1:2], in_=stf[:, 1:2], func=AF.Exp, scale=-0.5)
        op("dve", "scalar_tensor_tensor", [x_, stf, finw], [o_], out=o_[:], in0=x_[:], scalar=stf[:, 1:2], in1=finw[:],
           op0=ALU.mult, op1=ALU.mult)
        fw.dma("act", out_d[ti * 128:(ti + 1) * 128, :], o_[:], reads=[o_], writes=[out_tok])
    fw.finish([out_tok])
    print("ins", fw.n_ins, "waits", fw.n_wait, fw.seq)
    return nc


def make_in_map(b, x, c, ctx, c_ctx, ada_w, ada_b, norm_mix_w, norm_ffn_w, w_in, hgrn_lb_logits,
                hgrn_norm_w, sgu_norm_w, sgu_w, sgu_b, w_branch_a, w_branch_b, w_out, router_w,
                expert_w_gate, expert_w_up, expert_w_down, final_norm_w, T=None):
    f = lambda a: np.ascontiguousarray(np.asarray(a, dtype=np.float32))
    cc = np.stack([np.asarray(c)[b], np.asarray(c_ctx)], axis=0)
    ccT = cc.reshape(2, KD, 128).transpose(2, 1, 0)
    xb = np.asarray(x)[b]
    if T is not None:
        xb = xb[:T]
    return {
        "x": f(xb), "ctx": f(np.asarray(ctx)[b]), "ccT": f(ccT),
        "ada_w": f(np.asarray(ada_w)[0]), "ada_b": f(np.asarray(ada_b)[0].reshape(1, -1)),
        "nw": f(np.stack([np.asarray(norm_mix_w)[0], np.asarray(norm_ffn_w)[0], np.asarray(final_norm_w)], 0)),
        "w_in": f(np.asarray(w_in)[0]),
        "lbl": f(np.asarray(hgrn_lb_logits)[:, 0:2, :].reshape(1, -1)),
        "hnw": f(np.asarray(hgrn_norm_w)[0].reshape(1, -1)), "snw": f(np.asarray(sgu_norm_w)[0].reshape(1, -1)),
        "sgu_wT": f(np.asarray(sgu_w)[0].transpose(2, 0, 1)),
        "sgu_bT": f(np.asarray(sgu_b)[0].T),
        "w_a": f(np.asarray(w_branch_a)[0]), "w_b": f(np.asarray(w_branch_b)[0]), "w_o": f(np.asarray(w_out)[0]),
        "rw": f(np.asarray(router_w)[0]),
        "cst": make_consts(),
        **{"wg%d" % e: f(np.asarray(expert_w_gate)[0, e]) for e in range(NE)},
        **{"wu%d" % e: f(np.asarray(expert_w_up)[0, e]) for e in range(NE)},
        **{"wd%d" % e: f(np.asarray(expert_w_down)[0, e]) for e in range(NE)},
    }


def kernel(**inputs):
    B = np.asarray(inputs["x"]).shape[0]
    nc = build(64)
    n = 8
    in_maps = [make_in_map(i % B, **inputs) for i in range(n)]
    for i in range(1, n):
        for k in in_maps[0]:
            if k not in ("x", "ctx", "ccT"):
                in_maps[i][k] = in_maps[0][k]
    res = run_bass_kernel_spmd(nc, in_maps, core_ids=list(range(n)))
    out = np.stack([np.asarray(res.results[i]["out"]) for i in range(B)], axis=0)
    return out.astype(np.float32)
```

```python
import numpy as np
from contextlib import ExitStack
import concourse.bass as bass
import concourse.mybir as mybir
from concourse.bass_utils import run_bass_kernel_spmd

F32 = mybir.dt.float32
BF16 = mybir.dt.bfloat16
I32 = mybir.dt.int32
AF = mybir.ActivationFunctionType
ALU = mybir.AluOpType
AX = mybir.AxisListType

D = 1024
KD = 8
HW = 512
NH = 4
INC = 5632
NE = 16
FF = 2048
CTX = 256
EPS = 1e-6
GELU_C = 1.5957691216057308


class Buf:
    __slots__ = ("name", "t", "w", "r", "dsem", "multi", "dw", "dr")

    def __init__(self, name, t=None, multi=False):
        self.name = name
        self.t = t
        self.w = []
        self.r = {}
        self.dsem = None
        self.multi = multi
        self.dw = []
        self.dr = []

    def __getitem__(self, k):
        return self.t[k]


class DmaSem:
    def __init__(self, h):
        self.h = h
        self.val = 0


class FW:
    SAME_ENGINE_SYNC = ("act", "dve", "pool")
    PARTIAL_OK = False
    XLAT = 150.0
    TBL_NS = 1280.0

    def __init__(self, nc):
        self.nc = nc
        self.eng = {"pe": nc.tensor, "act": nc.scalar, "dve": nc.vector,
                    "pool": nc.gpsimd, "sp": nc.sync}
        self.psem = {k: nc.alloc_semaphore(name="p_" + k) for k in ("pe", "act", "dve", "pool")}
        self.seq = {k: 0 for k in self.psem}
        self.waited = {k: {} for k in self.eng}
        self.n_wait = 0
        self.n_ins = 0
        self.banks = []
        self.bank_i = 0
        self.defer = False
        self.recs = []
        self.touched = []

    def sb(self, name, shape, dt, dma=False, stack=None):
        if stack is None:
            t = self.nc.alloc_sbuf_tensor("s_" + name, list(shape), dt)
        else:
            t = stack.enter_context(self.nc.sbuf_tensor("s_" + name, list(shape), dt))
        b = Buf(name, t)
        if dma is True:
            b.dsem = self.new_dsem(name)
        elif dma:
            b.dsem = dma
        return b

    def token(self, name):
        return Buf(name, None, multi=True)

    def new_dsem(self, name):
        return DmaSem(self.nc.alloc_semaphore(name="d_" + name))

    def init_banks(self):
        for i in range(8):
            t = self.nc.alloc_psum_tensor("bank%d" % i, [128, 512], F32)
            self.banks.append(Buf("bank%d" % i, t))

    def bank(self):
        b = self.banks[self.bank_i % 8]
        self.bank_i += 1
        return b

    def _semh(self, key):
        return self.psem[key] if isinstance(key, str) else key.h

    def _wait(self, e, key, val):
        if val <= 0:
            return
        if isinstance(key, str) and key == e and e not in self.SAME_ENGINE_SYNC:
            return
        w = self.waited[e]
        if w.get(key, 0) >= val:
            return
        w[key] = val
        self.eng[e].wait_ge(self._semh(key), val)
        self.n_wait += 1

    def _deps(self, e, reads, writes, skip=None, partial=False, same=False):
        for b in reads:
            for (k, v) in b.w:
                self._wait(e, k, v)
        for b in writes:
            if not partial:
                for (k, v) in b.w:
                    if k is not skip and not (same and k == e):
                        self._wait(e, k, v)
            for k, v in b.r.items():
                self._wait(e, k, v)

    def _commit(self, ev, reads, writes, partial=False):
        k, v = ev
        for b in writes:
            if b.multi or partial:
                b.w = [x for x in b.w if x[0] != k] + [ev]
            else:
                b.w = [ev]
            if not partial:
                b.r = {}
        for b in reads:
            if b in writes:
                continue
            if b.r.get(k, 0) < v:
                b.r[k] = v

    def begin_defer(self):
        self.defer = True
        self.recs = []
        self.touched = []

    @staticmethod
    def _nfree(ap):
        try:
            n = 1
            for d in ap.shape[1:]:
                n *= int(d)
            return n
        except Exception:
            return 512

    def _est(self, e, name, a, kw):
        ap = kw.get("out", a[0] if a else None)
        n = self._nfree(ap) if ap is not None else 512
        if e == "pe":
            return 110.0 if name == "transpose" else 70.0 + 0.42 * n
        if e == "act":
            return 230.0 + 0.75 * n
        if e == "dve":
            return 200.0 + 0.95 * n
        return 300.0 + 1.9 * n

    def _record(self, e, fn, reads, writes, dur, sem=None, lat=0.0, extra=(), nowaw=False, tbl=None, same=False):
        R = [len(self.recs), e, fn, None, [], dur, sem, None, lat, tbl]
        deps = set()
        ext = R[4]
        for b in extra:
            deps.update(b.dw)
            ext.extend(b.w)
        for b in reads:
            deps.update(b.dw)
            ext.extend(b.w)
        for b in writes:
            if same:
                deps.update(d for d in b.dw if self.recs[d][1] != e)
                ext.extend(b.w)
            elif not (b.multi or nowaw):
                deps.update(b.dw)
                ext.extend(b.w)
            deps.update(b.dr)
            ext.extend(b.r.items())
        for b in writes:
            if b.multi or nowaw or same:
                b.dw.append(R[0])
            else:
                b.dw = [R[0]]
                b.w = []
                b.r = {}
            b.dr = []
            self.touched.append(b)
        for b in reads:
            if b not in writes:
                b.dr.append(R[0])
                self.touched.append(b)
        R[3] = sorted(deps)
        self.recs.append(R)
        return None

    def flush(self, window=48):
        recs = self.recs
        self.defer = False
        n = len(recs)
        if n == 0:
            return
        engs = ("pe", "act", "dve", "pool", "sp")
        XLAT = self.XLAT
        TBL = self.TBL_NS
        ndep = [len(r[3]) for r in recs]
        users = [[] for _ in range(n)]
        for r in recs:
            for d in r[3]:
                users[d].append(r[0])
        rdy = [0.0] * n
        fin = [0.0] * n
        start = [0.0] * n
        ready = {e: [] for e in engs}
        for r in recs:
            if ndep[r[0]] == 0:
                ready[r[1]].append(r[0])
        etime = {e: 0.0 for e in engs}
        cur_tbl = None
        order = []
        remaining = n
        sched = [False] * n
        low = 0
        LOOK = window
        while remaining:
            best = None
            while low < n and sched[low]:
                low += 1
            lim = low + LOOK
            for e in engs:
                lst = ready[e]
                if not lst:
                    continue
                et = etime[e]
                for rid in lst:
                    if rid >= lim:
                        continue
                    st = rdy[rid] if rdy[rid] > et else et
                    if e == "act":
                        tb = recs[rid][9]
                        if tb is not None and tb != cur_tbl:
                            st += TBL
                    key = (st, rid)
                    if best is None or key < best[0]:
                        best = (key, e, rid, st)
            assert best is not None, "list scheduler stuck"
            _, e, rid, st = best
            ready[e].remove(rid)
            sched[rid] = True
            start[rid] = st
            r = recs[rid]
            if r[9] is not None:
                cur_tbl = r[9]
            etime[e] = st + r[5]
            f_ = st + r[5] + r[8]
            fin[rid] = f_
            for u in users[rid]:
                fu = f_ if recs[u][1] == e else f_ + XLAT
                if fu > rdy[u]:
                    rdy[u] = fu
                ndep[u] -= 1
                if ndep[u] == 0:
                    ready[recs[u][1]].append(u)
            order.append(rid)
            remaining -= 1
        order.sort(key=lambda rid: (start[rid], rid))
        self.sched_span = max(fin) if fin else 0.0
        for rid in order:
            r = recs[rid]
            e = r[1]
            for d in r[3]:
                k, v = recs[d][7]
                self._wait(e, k, v)
            for (k, v) in r[4]:
                self._wait(e, k, v)
            ins = r[2]()
            if r[6] is None:
                self.seq[e] += 1
                ins.then_inc(self.psem[e], 1)
                r[7] = (e, self.seq[e])
            else:
                sem = r[6]
                sem.val += 16
                ins.then_inc(sem.h, 16)
                r[7] = (sem, sem.val)
            self.n_ins += 1
        seen = set()
        for b in self.touched:
            if id(b) in seen:
                continue
            seen.add(id(b))
            evs = {}
            for d in b.dw:
                k, v = recs[d][7]
                if evs.get(k, 0) < v:
                    evs[k] = v
            if b.dw:
                if b.multi:
                    old = {k: v for (k, v) in b.w}
                    old.update(evs)
                    b.w = list(old.items())
                else:
                    b.w = list(evs.items())
            for d in b.dr:
                k, v = recs[d][7]
                if b.r.get(k, 0) < v:
                    b.r[k] = v
            b.dw = []
            b.dr = []
        self.recs = []
        self.touched = []

    def op(self, e, name, reads, writes, *a, **kw):
        if getattr(self, "defer", False):
            kw.pop("partial", None)
            same = kw.pop("same_slice", False)
            eng = self.eng[e]
            fn = (lambda eng=eng, name=name, a=a, kw=kw: getattr(eng, name)(*a, **kw))
            tbl = None
            if e == "act":
                f_ = kw.get("func")
                if f_ == AF.Tanh:
                    tbl = "T"
                elif f_ == AF.Ln:
                    tbl = "L"
            return self._record(e, fn, list(reads), list(writes), self._est(e, name, a, kw), tbl=tbl, same=same)
        partial = kw.pop("partial", False) and self.PARTIAL_OK
        same = kw.pop("same_slice", False)
        self._deps(e, reads, writes, partial=partial, same=same)
        ins = getattr(self.eng[e], name)(*a, **kw)
        self.seq[e] += 1
        ins.then_inc(self.psem[e], 1)
        self._commit((e, self.seq[e]), reads, writes, partial=partial)
        self.n_ins += 1
        return ins

    def dma(self, q, out, in_, reads=(), writes=(), sem=None, indirect=None, extra=(), nowait_w=False, **kw):
        if getattr(self, "defer", False):
            if sem is None:
                for b in list(writes) + list(reads):
                    if b.dsem is not None:
                        sem = b.dsem
                        break
            assert sem is not None, "dma needs a semaphore"
            eng = self.eng[q]
            if indirect is not None:
                fn = (lambda: eng.indirect_dma_start(out=out, in_=in_, **indirect))
            else:
                fn = (lambda: eng.dma_start(out=out, in_=in_, **kw))
            nbytes = 4.0 * self._nfree(out) * 128
            return self._record(q, fn, list(reads), list(writes), 120.0, sem=sem, lat=2500.0 + nbytes / 150.0,
                                extra=extra, nowaw=nowait_w)
        for b in extra:
            for (k, v) in b.w:
                self._wait(q, k, v)
        if sem is None:
            for b in list(writes) + list(reads):
                if b.dsem is not None:
                    sem = b.dsem
                    break
        assert sem is not None, "dma needs a semaphore"
        self._deps(q, reads, writes, skip=sem, partial=nowait_w)
        if indirect is not None:
            ins = self.eng[q].indirect_dma_start(out=out, in_=in_, **indirect)
        else:
            ins = self.eng[q].dma_start(out=out, in_=in_, **kw)
        sem.val += 16
        ins.then_inc(sem.h, 16)
        self._commit((sem, sem.val), reads, writes)
        self.n_ins += 1
        return ins

    def finish(self, bufs):
        for b in bufs:
            for (k, v) in b.w:
                self._wait("sp", k, v)


C_IDENT, C_MF, C_MB, C_MASKF, C_MASKB, C_SELF, C_SELB, C_SEL, C_IOSI, C_IOSB, C_PV, C_JV, C_ONES, C_LSTR = \
    0, 128, 256, 384, 512, 640, 642, 644, 900, 1028, 1036, 1037, 1101, 1229
NCST = 1357


def make_consts():
    c = np.zeros((128, NCST), np.float32)
    s = np.arange(128)[:, None]
    t = np.arange(128)[None, :]
    c[:, C_IDENT:C_IDENT + 128] = np.eye(128)
    mf = np.where((t >= 64) & (s >= 64) & (s <= t), 1.0, 0.0) - np.where((t < 64) & (s > t) & (s <= 63), 1.0, 0.0)
    mb = np.where((t < 64) & (s >= t) & (s <= 63), 1.0, 0.0) - np.where((t >= 64) & (s >= 64) & (s < t), 1.0, 0.0)
    c[:, C_MF:C_MF + 128] = mf
    c[:, C_MB:C_MB + 128] = mb
    c[:, C_MASKF:C_MASKF + 128] = (s <= t)
    c[:, C_MASKB:C_MASKB + 128] = (s >= t)
    c[:, C_SELF] = (np.arange(128) <= 63)
    c[:, C_SELF + 1] = 1.0
    c[:, C_SELB] = (np.arange(128) >= 64)
    c[:, C_SELB + 1] = 1.0
    c[0, C_SEL:C_SEL + 128] = 1.0
    c[1, C_SEL + 128:C_SEL + 256] = 1.0
    c[:, C_IOSI:C_IOSI + 128] = np.arange(128)[None, :]
    c[:, C_IOSB:C_IOSB + 8] = np.arange(8)[None, :]
    c[:, C_PV] = np.arange(128) + 1
    c[:, C_JV:C_JV + 64] = np.arange(64)[None, :]
    c[:, C_ONES:C_ONES + 128] = 1.0
    c[:, C_LSTR:C_LSTR + 128] = (s < t)
    return c


def build(NT=64, debug=None, n_bisect=30, NSA=3, NSC=4, WIN=150):
    T = NT * 128
    CAP = 2 * T // NE
    SB = CAP // 128
    assert SB >= 1
    nc = bass.Bass("TRN2", target_bir_lowering=False)
    dt = lambda n, s, d=F32, kind="ExternalInput": nc.dram_tensor(n, list(s), d, kind=kind)
    x_d = dt("x", [T, D])
    ctx_d = dt("ctx", [CTX, D])
    cc_d = dt("ccT", [128, KD, 2])
    adaw_d = dt("ada_w", [D, 6 * D])
    adab_d = dt("ada_b", [1, 6 * D])
    nw_d = dt("nw", [3, D])
    win_d = dt("w_in", [D, INC])
    lbl_d = dt("lbl", [1, 2 * 2 * HW])
    hnw_d = dt("hnw", [1, HW])
    snw_d = dt("snw", [1, HW])
    sguw_d = dt("sgu_wT", [128, 4, 128])
    sgub_d = dt("sgu_bT", [128, 4])
    wa_d = dt("w_a", [HW, D])
    wb_d = dt("w_b", [HW, D])
    wo_d = dt("w_o", [D, D])
    rw_d = dt("rw", [D, NE])
    if debug not in ("xmid", "route"):
        wg_l = [dt("wg%d" % e, [D, FF]) for e in range(NE)]
        wu_l = [dt("wu%d" % e, [D, FF]) for e in range(NE)]
        wd_l = [dt("wd%d" % e, [FF, D]) for e in range(NE)]
    cst_d = dt("cst", [128, NCST])
    out_d = dt("out", [T, D], F32, "ExternalOutput")
    if debug == "xmid":
        xacc_d = dt("xacc", [T, D], F32, "ExternalOutput")
        h2_d = dt("h2d", [T, D], BF16, "ExternalOutput")
        aff_dbg = dt("affd", [128, NT, NE], F32, "ExternalOutput")
    else:
        xacc_d = nc.dram_tensor("xacc", [T, D], F32)
        h2_d = nc.dram_tensor("h2d", [T, D], BF16)
    if debug == "route":
        aff_dbg = dt("affd", [128, NT, NE], F32, "ExternalOutput")
        idx_dbg = dt("idxd", [128, NE, SB], I32, "ExternalOutput")
        gate_dbg = dt("gated", [128, NE, SB], F32, "ExternalOutput")
    ob_d = nc.dram_tensor("obd", [T, HW], BF16)

    mods_d = nc.dram_tensor("mods", [2, 6, D], F32)
    fw = FW(nc)
    fw.init_banks()
    op = fw.op
    for bk_ in fw.banks:
        op("dve", "memset", [], [bk_], bk_[:, :], 0.0)
    S0 = ExitStack()
    S1 = ExitStack()

    setup_sems = {"hw": fw.new_dsem("setup"), "pool": fw.new_dsem("setup_sw")}
    setup_bufs = []

    def sload(q, name, shape, dtp, src, stack=S0):
        sem_ = setup_sems["pool" if q == "pool" else "hw"]
        b = fw.sb(name, shape, dtp, dma=sem_, stack=stack)
        fw.dma(q, b[:], src, writes=[b])
        setup_bufs.append(b)
        return b

    sf_count = [0]

    def sfinal():
        for b in setup_bufs:
            b.w = [(b.dsem, b.dsem.val)]
        del setup_bufs[:]
        sf_count[0] += 1
        setup_sems["hw"] = fw.new_dsem("setup%d" % sf_count[0])
        setup_sems["pool"] = fw.new_dsem("setup_sw%d" % sf_count[0])

    NCA = C_SEL
    cst = sload("sp", "cst", [128, NCA], F32, cst_d[:, 0:NCA])
    identb = sload("pool", "identb", [128, 128], BF16, cst_d[:, C_IDENT:C_IDENT + 128])
    maski = fw.sb("maski", [128, 2, 4, 128], I32, dma=setup_sems["pool"], stack=S0)
    for d_ in range(2):
        fw.dma("pool", maski[:, d_, :, :],
               cst_d[:, C_MASKF + 128 * d_:C_MASKF + 128 * (d_ + 1)].unsqueeze(1).to_broadcast([128, 4, 128]),
               writes=[maski])
    setup_bufs.append(maski)
    mhalf = fw.sb("mhalf", [128, 8], F32, stack=S0)
    fw.op("pool", "memset", [], [mhalf], mhalf[:], -0.5)
    AFF = fw.sb("AFF", [128, NT, NE], F32, stack=S0)
    RS = fw.sb("RS", [128, NT], F32, stack=S0)
    lb = fw.sb("lb", [128, 2, HW], F32, stack=S0)
    oml = fw.sb("oml", [128, 2, HW], F32, stack=S0)
    colv = fw.sb("colv", [128, 2, 6, KD], F32, dma=True, stack=S0)
    ident = cst[:, C_IDENT:C_IDENT + 128]
    mods_tok = fw.token("mods_tok")

    NA = 3584
    win = fw.sb("winA", [128, KD, NA], BF16, dma=True, stack=S1)
    for k in range(KD):
        fw.dma("pool", win[:, k, :], win_d[k * 128:(k + 1) * 128, 0:NA], writes=[win])
    with ExitStack() as SA:
        lbl = sload("sp", "lbl", [128, 2, 2, HW], F32,
                    lbl_d.ap().to_broadcast([128, 2 * 2 * HW]).rearrange("p (a b c) -> p a b c", a=2, b=2), stack=SA)
        nw2 = sload("sp", "nw2", [2, 2, D], F32,
                    nw_d[0:2, :].rearrange("(o a) d -> o a d", o=1).to_broadcast([2, 2, D]), stack=SA)
        adab2 = sload("sp", "adab2", [2, 6 * D], F32, adab_d.ap().to_broadcast([2, 6 * D]), stack=SA)
        ccT = sload("sp", "ccT", [128, KD, 2], F32, cc_d.ap(), stack=SA)
        sfinal()
        op("dve", "tensor_tensor", [lbl], [lb], out=lb[:], in0=lbl[:, :, 0, :], in1=lbl[:, :, 1, :], op=ALU.subtract)
        op("act", "activation", [lb], [lb], out=lb[:], in_=lb[:], func=AF.Sigmoid)
        op("dve", "tensor_scalar", [lb], [oml], out=oml[:], in0=lb[:], scalar1=-0.5, scalar2=0.5,
           op0=ALU.mult, op1=ALU.add)
        op("dve", "tensor_tensor", [lb, oml], [lb], out=lb[:], in0=lb[:], in1=oml[:], op=ALU.add)
        scc = fw.sb("scc", [128, KD, 2], F32, stack=SA)
        msb = fw.sb("msb", [2, 6 * D], F32, stack=SA)
        vec = fw.sb("vec", [2, 6, D], F32, dma=True, stack=SA)
        awb = [fw.sb("awb%d" % i, [128, KD, 512], F32, dma=True, stack=SA) for i in range(2)]
        op("act", "activation", [ccT], [scc], out=scc[:], in_=ccT[:], func=AF.Sigmoid)
        op("dve", "tensor_tensor", [scc, ccT], [scc], out=scc[:], in0=scc[:], in1=ccT[:], op=ALU.mult)
        for n in range(12):
            a = awb[n % 2]
            fw.dma("sp" if n % 2 == 0 else "act", a[:],
                   adaw_d[:, n * 512:(n + 1) * 512].rearrange("(k p) n -> p k n", p=128), writes=[a])
            bk = fw.bank()
            for k in range(KD):
                op("pe", "matmul", [scc, a], [bk], bk[0:2, :], lhsT=scc[:, k, :], rhs=a[:, k, :],
                   start=(k == 0), stop=(k == KD - 1))
            op("dve", "tensor_tensor", [bk, adab2], [msb], out=msb[:, n * 512:(n + 1) * 512], in0=bk[0:2, :],
               in1=adab2[:, n * 512:(n + 1) * 512], op=ALU.add)
        op("dve", "scalar_tensor_tensor", [msb, nw2], [vec], out=vec[:, 0, :], in0=msb[:, D:2 * D], scalar=1.0,
           in1=nw2[:, 0, :], op0=ALU.add, op1=ALU.mult)
        op("dve", "tensor_copy", [msb], [vec], out=vec[:, 1, :], in_=msb[:, 0:D])
        op("dve", "tensor_copy", [msb], [vec], out=vec[:, 2, :], in_=msb[:, 2 * D:3 * D])
        op("dve", "scalar_tensor_tensor", [msb, nw2], [vec], out=vec[:, 3, :], in0=msb[:, 4 * D:5 * D], scalar=1.0,
           in1=nw2[:, 1, :], op0=ALU.add, op1=ALU.mult)
        op("dve", "tensor_copy", [msb], [vec], out=vec[:, 4, :], in_=msb[:, 3 * D:4 * D])
        op("dve", "tensor_copy", [msb], [vec], out=vec[:, 5, :], in_=msb[:, 5 * D:6 * D])
        fw.dma("sp", mods_d.ap(), vec[:], reads=[vec], writes=[mods_tok])
        with nc.allow_non_contiguous_dma(reason="tiny column-layout load"):
            for r_ in range(2):
                fw.dma("sp", colv[:, r_, :, :], mods_d[r_, :, :].rearrange("v (k p) -> p v k", p=128),
                       reads=[mods_tok], writes=[colv])
        fw.finish([colv, mods_tok])

    def barrier():
        for e in ("pe", "act", "dve", "pool", "sp"):
            for k in ("pe", "act", "dve", "pool"):
                if k != e or e in fw.SAME_ENGINE_SYNC:
                    fw._wait(e, k, fw.seq[k])

    barrier()

    oa_d = nc.dram_tensor("oad", [T, HW], BF16)
    sg_d = nc.dram_tensor("sgd", [T, HW], BF16)
    xacc_tok = fw.token("xacc_tok")
    h2_tok = fw.token("h2_tok")
    ob_tok = fw.token("ob_tok")
    oa_tok = fw.token("oa_tok")
    q_d = nc.dram_tensor("qd", [T, HW], BF16)
    v_d = nc.dram_tensor("vd", [T, HW], BF16)
    q_tok = fw.token("q_tok")
    v_tok = fw.token("v_tok")
    sg_tok = fw.token("sg_tok")

    def V3(buf):
        return buf[:].rearrange("p (h c) -> p h c", h=4)

    def bc4(t):
        return t[:].unsqueeze(2).to_broadcast([128, 4, 128])

    def pipeline(gens, depth=2):
        active = []
        nxt = 0
        while nxt < len(gens) or active:
            if nxt < len(gens) and len(active) < depth and (not active or active[-1][1]):
                active.append([gens[nxt], False])
                nxt += 1
            for a_ in list(active):
                try:
                    m = next(a_[0])
                    if m == "H":
                        a_[1] = True
                except StopIteration:
                    active.remove(a_)

    def proj(lhs, w, kn, c0, ncols=512):
        bk = fw.bank()
        for k in range(kn):
            op("pe", "matmul", [lhs, w], [bk], bk[:, 0:ncols], lhsT=lhs[:, k, :], rhs=w[:, k, c0:c0 + ncols],
               start=(k == 0), stop=(k == kn - 1))
        return bk

    def rstd_from_ss(ss_ap, n, out_ap, rbufs, wbufs, eb=None):
        eb = epsb if eb is None else eb
        op("act", "activation", rbufs + [eb], wbufs, out=out_ap, in_=ss_ap, func=AF.Ln, scale=1.0 / n, bias=eb[:, 0:1])
        op("act", "activation", wbufs, wbufs, out=out_ap, in_=out_ap, func=AF.Exp, scale=-0.5)

    def norm_T(xtile, hb, hT, st, row, rs_ap=None, rs_buf=None, rs_out=None):
        if rs_ap is None:
            op("act", "activation", [xtile], [hb, st], out=hb[:], in_=xtile[:], func=AF.Square, accum_out=st[:, 0:1])
            rstd_from_ss(st[:, 0:1], D, st[:, 1:2], [st], [st])
            if rs_out is not None:
                op("dve", "tensor_copy", [st], [RS], out=rs_out, in_=st[:, 1:2])
            rs_ap, rs_buf = st[:, 1:2], st
        op("dve", "tensor_scalar", [xtile, rs_buf], [hb], out=hb[:], in0=xtile[:], scalar1=rs_ap, scalar2=None,
           op0=ALU.mult)
        bk = fw.bank()
        v = bk[:].bitcast(BF16).rearrange("p (a b) -> p a b", a=8)
        for k in range(KD):
            op("pe", "transpose", [hb, identb], [bk], v[:, k, :], hb[:, k * 128:(k + 1) * 128], identb[:])
        for k in range(KD):
            if k % 2 == 0:
                op("act", "activation", [bk, colv], [hT], out=hT[:, k, :], in_=v[:, k, :], func=AF.Identity,
                   scale=colv[:, row, 0, k:k + 1], bias=colv[:, row, 1, k:k + 1], partial=True)
            else:
                op("dve", "tensor_scalar", [bk, colv], [hT], out=hT[:, k, :], in0=v[:, k, :],
                   scalar1=colv[:, row, 0, k:k + 1], scalar2=colv[:, row, 1, k:k + 1], op0=ALU.mult, op1=ALU.add,
                   partial=True)

    def transpose_to(src, dst, n, eng="act"):
        bk = fw.bank()
        v = bk[:].bitcast(BF16).rearrange("p (a b) -> p a b", a=8)
        for k in range(n):
            op("pe", "transpose", [src, identb], [bk], v[:, k, :], src[:, k * 128:(k + 1) * 128], identb[:])
        if eng == "act":
            op("act", "activation", [bk], [dst], out=dst[:, 0:n, :], in_=v[:, 0:n, :], func=AF.Copy)
        else:
            op("dve", "tensor_copy", [bk], [dst], out=dst[:, 0:n, :], in_=v[:, 0:n, :])

    def silu2_from_bank(bk, tmp, dst):
        op("act", "activation", [bk], [tmp], out=tmp[:], in_=bk[:, :], func=AF.Tanh, scale=0.5)
        op("dve", "scalar_tensor_tensor", [bk, tmp], [dst], out=dst[:], in0=tmp[:], scalar=1.0, in1=bk[:, :],
           op0=ALU.add, op1=ALU.mult)

    def tanh_half_from_bank(bk, dst):
        op("act", "activation", [bk], [dst], out=dst[:], in_=bk[:, :], func=AF.Tanh, scale=0.5)

    hnw = sload("sp", "hnw", [128, HW], F32, hnw_d.ap().to_broadcast([128, HW]), stack=S1)
    snw = sload("sp", "snw", [128, HW], F32, snw_d.ap().to_broadcast([128, HW]), stack=S1)
    sguw = sload("pool", "sguw", [128, 4, 128], BF16, sguw_d.ap(), stack=S1)
    sgub = sload("sp", "sgub", [128, 4], F32, sgub_d.ap(), stack=S1)
    sfinal()
    op("dve", "tensor_scalar", [hnw], [hnw], out=hnw[:], in0=hnw[:], scalar1=0.5, scalar2=None, op0=ALU.mult)
    op("dve", "tensor_scalar", [sguw], [sguw], out=sguw[:], in0=sguw[:], scalar1=0.5, scalar2=None, op0=ALU.mult)
    op("dve", "tensor_scalar", [sgub], [sgub], out=sgub[:], in0=sgub[:], scalar1=0.5, scalar2=None, op0=ALU.mult)
    W = lambda name, shape, dtp, dma=False: fw.sb(name, shape, dtp, dma=dma, stack=S1)
    Sst = [W("S%d" % i, [128, 4, 128], F32) for i in range(2)]
    epsb = W("epsb", [128, 1], F32)
    for s_ in Sst:
        op("pool", "memset", [], [s_], s_[:], 0.0)
    op("pool", "memset", [], [epsb], epsb[:], EPS)
    epsb4 = W("epsb4", [128, 1], F32)
    op("pool", "memset", [], [epsb4], epsb4[:], 4.0 * EPS)
    lnh = W("lnh", [128, 1], F32)
    op("pool", "memset", [], [lnh], lnh[:], -0.6931471805599453)

    class Set:
        pass

    def make_setA(i):
        z = Set()
        n = lambda nm: "%s_%d" % (nm, i)
        z.xt = W(n("xt"), [128, D], F32, dma=True)
        z.obt = W(n("obt"), [128, HW], BF16, dma=True)
        z.st = [W(n("st%d" % j), [128, 8], F32) for j in range(3)]
        z.F = [W(n("F%d" % j), [128, HW], F32) for j in range(6)]
        z.H = [W(n("H%d" % j), [128, HW], BF16, dma=(j in (0, 1, 2))) for j in range(6)]
        z.hb = W(n("hb"), [128, D], BF16)
        z.hT = W(n("hT"), [128, KD, 128], BF16)
        z.QKT = W(n("QKT"), [128, 8, 128], BF16)
        z.bml = W(n("bml"), [128, 4, 2], F32)
        z.ebm = W(n("ebm"), [128, 4], F32)
        z.edl = W(n("edl"), [128, 4], F32)
        z.oast = W(n("oast"), [128, HW], BF16, dma=True)
        z.sgst = W(n("sgst"), [128, HW], BF16, dma=True)
        z.gw = W(n("gw"), [128, HW], BF16)
        z.ug = W(n("ug"), [128, HW], BF16)
        z.vs = W(n("vs"), [128, HW], F32)
        z.gp = W(n("gp"), [128, HW], F32)
        return z

    setsA = [make_setA(i) for i in range(NSA)]

    def hgrn_tile(z, d, want_o):
        sig, ff, logf, kk, Epos, Eneg = z.F
        Sp = z.F[5]
        qs, vv, Qt, Kt, Spb, sT = z.H
        QKT, bml, ebm, edl = z.QKT, z.bml, z.ebm, z.edl
        S = Sst[d]
        Mm = cst[:, (C_MF if d == 0 else C_MB):(C_MF if d == 0 else C_MB) + 128]
        selc2 = cst[:, (C_SELF if d == 0 else C_SELB):(C_SELF if d == 0 else C_SELB) + 2]
        op("pool", "tensor_tensor", [sig, oml], [ff], out=ff[:], in0=sig[:], in1=oml[:, d, :], op=ALU.mult)
        op("pool", "tensor_tensor", [ff, lb], [ff], out=ff[:], in0=ff[:], in1=lb[:, d, :], op=ALU.add)
        op("act", "activation", [ff], [logf], out=logf[:], in_=ff[:], func=AF.Ln)
        op("pool", "tensor_scalar", [ff], [kk], out=kk[:], in0=ff[:], scalar1=-1.0, scalar2=1.0,
           op0=ALU.mult, op1=ALU.add)
        yield
        bb = fw.bank()
        op("pe", "matmul", [cst, logf], [bb], bb[:, :], lhsT=Mm, rhs=logf[:], start=True, stop=True)
        bl = fw.bank()
        blv = bl[:, 0:8].rearrange("p (h c) -> p h c", h=4)
        for h in range(NH):
            op("pe", "matmul", [logf, cst], [bl], blv[:, h, :], lhsT=logf[:, h * 128:(h + 1) * 128], rhs=selc2,
               start=True, stop=True)
        op("act", "activation", [bb], [Eneg], out=Eneg[:], in_=bb[:, :], func=AF.Exp, scale=-1.0)
        if want_o:
            op("act", "activation", [bb], [Epos], out=Epos[:], in_=bb[:, :], func=AF.Exp, bias=lnh[:, 0:1])
        op("dve", "tensor_copy", [bl], [bml], out=bml[:], in_=blv)
        yield
        op("pool", "tensor_tensor", [kk, Eneg], [Kt], out=Kt[:], in0=kk[:], in1=Eneg[:], op=ALU.mult)
        if want_o:
            op("pool", "tensor_tensor", [qs, Epos], [Qt], out=Qt[:], in0=qs[:], in1=Epos[:], op=ALU.mult)
        op("act", "activation", [bml], [ebm], out=ebm[:], in_=bml[:, :, 0], func=AF.Exp)
        op("dve", "tensor_tensor", [bml], [edl], out=edl[:], in0=bml[:, :, 1], in1=bml[:, :, 0], op=ALU.subtract)
        op("act", "activation", [edl], [edl], out=edl[:], in_=edl[:], func=AF.Exp)
        yield "H"
        op("dve", "tensor_tensor", [S, ebm], [Sp], out=V3(Sp), in0=S[:], in1=bc4(ebm), op=ALU.mult)
        z.ob = None
        if want_o:
            op("act", "activation", [Sp], [Spb], out=Spb[:], in_=Sp[:], func=AF.Copy)
            bk = fw.bank()
            v = bk[:].bitcast(BF16).rearrange("p (a b) -> p a b", a=8)
            for h in range(NH):
                op("pe", "transpose", [Qt, identb], [bk], v[:, h, :], Qt[:, h * 128:(h + 1) * 128], identb[:])
            for h in range(NH):
                op("pe", "transpose", [Kt, identb], [bk], v[:, 4 + h, :], Kt[:, h * 128:(h + 1) * 128], identb[:])
            op("act", "activation", [bk], [QKT], out=QKT[:], in_=v, func=AF.Copy)
            yield
            sc = fw.bank()
            scv = sc[:, :].rearrange("p (h c) -> p h c", h=4)
            for h in range(NH):
                if d == 0:
                    op("pe", "matmul", [QKT], [sc], scv[0:64, h, :], lhsT=QKT[:, 4 + h, 0:64], rhs=QKT[:, h, :],
                       start=True, stop=True)
                    op("pe", "matmul", [QKT], [sc], scv[64:128, h, 64:128], lhsT=QKT[:, 4 + h, 64:128],
                       rhs=QKT[:, h, 64:128], start=True, stop=True)
                else:
                    op("pe", "matmul", [QKT], [sc], scv[0:64, h, 0:64], lhsT=QKT[:, 4 + h, 0:64],
                       rhs=QKT[:, h, 0:64], start=True, stop=True)
                    op("pe", "matmul", [QKT], [sc], scv[64:128, h, :], lhsT=QKT[:, 4 + h, 64:128], rhs=QKT[:, h, :],
                       start=True, stop=True)
            op("pool", "memset", [], [sT], sT[:], 0.0)
            op("dve", "copy_predicated", [sc, maski], [sT], V3(sT), maski[:, d, :, :], scv)
            yield
        ub = fw.bank()
        for h in range(NH):
            hs = slice(h * 128, (h + 1) * 128)
            op("pe", "matmul", [Kt, vv], [ub], ub[:, hs], lhsT=Kt[:, hs], rhs=vv[:, hs], start=True, stop=True)
        op("dve", "tensor_tensor", [ub, Sp], [Sp], out=Sp[:], in0=ub[:, :], in1=Sp[:], op=ALU.add)
        if want_o:
            ob = fw.bank()
            obv = ob[:, :].rearrange("p (h c) -> p h c", h=4)
            for h in range(NH):
                hs = slice(h * 128, (h + 1) * 128)
                op("pe", "matmul", [sT, vv], [ob], obv[:, h, :], lhsT=sT[:, hs], rhs=vv[:, hs],
                   start=True, stop=False)
                op("pe", "matmul", [QKT, Spb], [ob], obv[:, h, :], lhsT=QKT[:, h, :], rhs=Spb[:, hs],
                   start=False, stop=True)
            z.ob = ob
        op("dve", "tensor_tensor", [Sp, edl], [S], out=S[:], in0=V3(Sp), in1=bc4(edl), op=ALU.mult)

    def gelu2_from_bank(z, bk, dst):
        gp = z.gp
        op("act", "activation", [bk], [gp], out=gp[:], in_=bk[:, :], func=AF.Square, scale=0.044715 ** 0.5)
        op("dve", "scalar_tensor_tensor", [gp, bk], [gp], out=gp[:], in0=gp[:], scalar=1.0, in1=bk[:, :],
           op0=ALU.add, op1=ALU.mult)
        op("act", "activation", [gp], [gp], out=gp[:], in_=gp[:], func=AF.Tanh, scale=0.5 * GELU_C)
        op("dve", "scalar_tensor_tensor", [gp, bk], [dst], out=dst[:], in0=gp[:], scalar=1.0, in1=bk[:, :],
           op0=ALU.add, op1=ALU.mult)

    def group_rms(z, buf, rs_tile, eb=None):
        sq = z.F[0]
        op("pool", "tensor_tensor", [buf], [sq], out=sq[:], in0=buf[:], in1=buf[:], op=ALU.mult)
        op("dve", "tensor_reduce", [sq], [rs_tile], out=rs_tile[:, 4:8], in_=V3(sq), axis=AX.X, op=ALU.add)
        rstd_from_ss(rs_tile[:, 4:8], 128, rs_tile[:, 0:4], [rs_tile], [rs_tile], eb)

    def body_ctx(z, ti, d):
        fw.dma("sp", z.xt[:], ctx_d[ti * 128:(ti + 1) * 128, :], writes=[z.xt])
        norm_T(z.xt, z.hb, z.hT, z.st[0], 1)
        bz = proj(z.hT, win, KD, 512 * (1 + d))
        tanh_half_from_bank(bz, z.F[0])
        bv = proj(z.hT, win, KD, 512 * 3)
        op("dve", "tensor_copy", [bv], [z.H[1]], out=z.H[1][:], in_=bv[:, :])
        for _ in hgrn_tile(z, d, False):
            pass
        yield "H"

    def body_p1(z, ti):
        fw.dma("sp", z.xt[:], x_d[ti * 128:(ti + 1) * 128, :], writes=[z.xt])
        norm_T(z.xt, z.hb, z.hT, z.st[0], 0, rs_out=RS[:, ti:ti + 1])
        yield
        bq = proj(z.hT, win, KD, 0)
        silu2_from_bank(bq, z.F[0], z.H[0])
        fw.dma("sp", q_d[ti * 128:(ti + 1) * 128, :], z.H[0][:], reads=[z.H[0]], writes=[q_tok])
        yield
        bz = proj(z.hT, win, KD, 512 * 2)
        tanh_half_from_bank(bz, z.F[0])
        bv = proj(z.hT, win, KD, 512 * 3)
        op("dve", "tensor_copy", [bv], [z.H[1]], out=z.H[1][:], in_=bv[:, :])
        fw.dma("sp", v_d[ti * 128:(ti + 1) * 128, :], z.H[1][:], reads=[z.H[1]], writes=[v_tok])
        yield
        for m_ in hgrn_tile(z, 1, True):
            yield m_
        obf = z.H[2]
        op("act", "activation", [z.ob], [obf], out=obf[:], in_=z.ob[:, :], func=AF.Copy)
        fw.dma("sp", ob_d[ti * 128:(ti + 1) * 128, :], obf[:], reads=[obf], writes=[ob_tok])
        yield

    def body_p2(z, ti):
        fw.dma("act", z.obt[:], ob_d[ti * 128:(ti + 1) * 128, :], reads=[ob_tok], writes=[z.obt])
        fw.dma("sp", z.xt[:], x_d[ti * 128:(ti + 1) * 128, :], writes=[z.xt])
        norm_T(z.xt, z.hb, z.hT, z.st[0], 0, rs_ap=RS[:, ti:ti + 1], rs_buf=RS)
        yield
        fw.dma("act", z.H[0][:], q_d[ti * 128:(ti + 1) * 128, :], reads=[q_tok], writes=[z.H[0]])
        fw.dma("act", z.H[1][:], v_d[ti * 128:(ti + 1) * 128, :], reads=[v_tok], writes=[z.H[1]])
        bg = proj(z.hT, win, KD, 512 * 4)
        silu2_from_bank(bg, z.gp, z.vs)
        op("pool", "tensor_tensor", [z.vs, hnw], [z.gw], out=z.gw[:], in0=z.vs[:], in1=hnw[:], op=ALU.mult)
        yield
        bu = proj(z.hT, win, KD, 512 * 5)
        gelu2_from_bank(z, bu, z.ug)
        yield
        bs = proj(z.hT, win, KD, 512 * 6)
        gelu2_from_bank(z, bs, z.vs)
        yield
        bz = proj(z.hT, win, KD, 512 * 1)
        tanh_half_from_bank(bz, z.F[0])
        yield
        for m_ in hgrn_tile(z, 0, True):
            yield m_
        osum = z.F[4]
        op("dve", "tensor_tensor", [z.ob, z.obt], [osum], out=osum[:], in0=z.ob[:, :], in1=z.obt[:], op=ALU.add)
        yield
        group_rms(z, osum, z.st[1])
        for h in range(NH):
            hs = slice(h * 128, (h + 1) * 128)
            op("dve", "scalar_tensor_tensor", [osum, z.st[1], z.gw], [z.oast], out=z.oast[:, hs], in0=osum[:, hs],
               scalar=z.st[1][:, h:h + 1], in1=z.gw[:, hs], op0=ALU.mult, op1=ALU.mult, same_slice=(h > 0))
        fw.dma("sp", oa_d[ti * 128:(ti + 1) * 128, :], z.oast[:], reads=[z.oast], writes=[oa_tok])
        yield
        vs, vn, ug = z.vs, z.H[5], z.ug
        group_rms(z, vs, z.st[2], epsb4)
        for g in range(4):
            hs = slice(g * 128, (g + 1) * 128)
            op("dve", "scalar_tensor_tensor", [vs, z.st[2], snw], [vn], out=vn[:, hs], in0=vs[:, hs],
               scalar=z.st[2][:, g:g + 1], in1=snw[:, hs], op0=ALU.mult, op1=ALU.mult, same_slice=(g > 0))
        yield
        mb_ = fw.bank()
        for g in range(4):
            hs = slice(g * 128, (g + 1) * 128)
            op("pe", "matmul", [sguw, vn], [mb_], mb_[:, hs], lhsT=sguw[:, g, :], rhs=vn[:, hs], start=True, stop=True)
        for g in range(4):
            hs = slice(g * 128, (g + 1) * 128)
            op("dve", "scalar_tensor_tensor", [mb_, sgub, ug], [z.sgst], out=z.sgst[:, hs], in0=mb_[:, hs],
               scalar=sgub[:, g:g + 1], in1=ug[:, hs], op0=ALU.add, op1=ALU.mult, same_slice=(g > 0))
        fw.dma("sp", sg_d[ti * 128:(ti + 1) * 128, :], z.sgst[:], reads=[z.sgst], writes=[sg_tok])
        yield

    gens = []
    for d in (0, 1):
        for ti in ((0, 1) if d == 0 else (1, 0)):
            gens.append(body_ctx(setsA[len(gens) % 2], ti, d))
    fw.begin_defer()
    pipeline(gens)
    fw.flush(WIN)
    fw.begin_defer()
    pipeline([body_p1(setsA[i % NSA], ti) for i, ti in enumerate(reversed(range(NT)))], NSA)
    fw.flush(WIN)
    fw.begin_defer()
    pipeline([body_p2(setsA[ti % NSA], ti) for ti in range(NT)], NSA)

    fw.flush(WIN)
    barrier()
    S1.close()

    S1 = ExitStack()
    W = lambda name, shape, dtp, dma=False: fw.sb(name, shape, dtp, dma=dma, stack=S1)
    rw = sload("sp", "rw", [128, KD, NE], F32, rw_d.ap().rearrange("(k p) e -> p k e", p=128), stack=S1)
    Gm = sload("sp", "Gm", [128, D], F32, mods_d[0, 2:3, :].to_broadcast([128, D]), stack=S1)
    Af = sload("sp", "Af", [128, D], F32, mods_d[0, 3:4, :].to_broadcast([128, D]), stack=S1)
    Bf = sload("sp", "Bf", [128, D], F32, mods_d[0, 4:5, :].to_broadcast([128, D]), stack=S1)
    sfinal()
    op("dve", "tensor_scalar", [Gm], [Gm], out=Gm[:], in0=Gm[:], scalar1=0.5, scalar2=None, op0=ALU.mult)
    winB = W("winB", [128, KD, 2048], BF16, dma=True)
    for k in range(KD):
        fw.dma("pool", winB[:, k, :], win_d[k * 128:(k + 1) * 128, NA:INC], writes=[winB])
    wa = W("wa", [128, 4, D], BF16, dma=True)
    fw.dma("pool", wa[:], wa_d.ap().rearrange("(k p) n -> p k n", p=128), writes=[wa])
    wb = W("wb", [128, 4, D], BF16, dma=True)
    fw.dma("pool", wb[:], wb_d.ap().rearrange("(k p) n -> p k n", p=128), writes=[wb])
    wo = W("wo", [128, KD, D], BF16, dma=True)
    fw.dma("pool", wo[:], wo_d.ap().rearrange("(k p) n -> p k n", p=128), writes=[wo])
    epsb = W("epsb3", [128, 1], F32)
    op("pool", "memset", [], [epsb], epsb[:], EPS)

    def make_setC(i):
        z = Set()
        n = lambda nm: "%s_c%d" % (nm, i)
        z.xt = W(n("xt"), [128, D], F32, dma=True)
        z.oat = W(n("oat"), [128, HW], BF16, dma=True)
        z.sgt = W(n("sgt"), [128, HW], BF16, dma=True)
        z.st = [W(n("st%d" % j), [128, 8], F32) for j in range(2)]
        z.big = W(n("big"), [128, D], BF16, dma=True)
        z.hT = W(n("hT"), [128, KD, 128], BF16)
        z.tT = W(n("tT"), [128, KD, 128], BF16)
        z.sgab = W(n("sgab"), [128, D], BF16)
        z.m1 = W(n("m1"), [128, D], F32)
        z.tmp = [W(n("tmp%d" % j), [128, HW], F32) for j in range(2)]
        z.lg = W(n("lg"), [128, NE], F32)
        return z

    setsC = [make_setC(i) for i in range(NSC)]

    def body_p3(z, ti):
        xtile, m1, sgab, big = z.xt, z.m1, z.sgab, z.big
        fw.dma("act", z.oat[:], oa_d[ti * 128:(ti + 1) * 128, :], reads=[oa_tok], writes=[z.oat])
        fw.dma("act", z.sgt[:], sg_d[ti * 128:(ti + 1) * 128, :], reads=[sg_tok], writes=[z.sgt])
        fw.dma("sp", xtile[:], x_d[ti * 128:(ti + 1) * 128, :], writes=[xtile])
        norm_T(xtile, big, z.hT, z.st[0], 0, rs_ap=RS[:, ti:ti + 1], rs_buf=RS)
        yield
        transpose_to(z.oat, z.tT, 4, eng="dve")
        ya = [proj(z.tT, wa, 4, 0), proj(z.tT, wa, 4, 512)]
        for hh in range(2):
            bga = proj(z.hT, winB, KD, hh * 512)
            op("act", "activation", [bga], [sgab], out=sgab[:, hh * 512:(hh + 1) * 512], in_=bga[:, :], func=AF.Tanh,
               scale=0.5)
        for hh in range(2):
            cs = slice(hh * 512, (hh + 1) * 512)
            op("dve", "scalar_tensor_tensor", [ya[hh], sgab], [m1], out=m1[:, cs], in0=sgab[:, cs], scalar=1.0,
               in1=ya[hh][:, :], op0=ALU.add, op1=ALU.mult)
        yield
        transpose_to(z.sgt, z.tT, 4, eng="dve")
        yb = [proj(z.tT, wb, 4, 0), proj(z.tT, wb, 4, 512)]
        for hh in range(2):
            bgb = proj(z.hT, winB, KD, 1024 + hh * 512)
            op("act", "activation", [bgb], [sgab], out=sgab[:, hh * 512:(hh + 1) * 512], in_=bgb[:, :], func=AF.Tanh,
               scale=0.5)
        for hh in range(2):
            cs = slice(hh * 512, (hh + 1) * 512)
            tmp = z.tmp[hh]
            op("dve", "scalar_tensor_tensor", [yb[hh], sgab], [tmp], out=tmp[:], in0=sgab[:, cs], scalar=1.0,
               in1=yb[hh][:, :], op0=ALU.add, op1=ALU.mult)
            op("dve", "tensor_tensor", [tmp, m1], [big], out=big[:, cs], in0=tmp[:], in1=m1[:, cs], op=ALU.add)
        yield
        transpose_to(big, z.tT, 8, eng="act")
        yo = [proj(z.tT, wo, KD, 0), proj(z.tT, wo, KD, 512)]
        for hh in range(2):
            cs = slice(hh * 512, (hh + 1) * 512)
            op("dve", "tensor_tensor", [yo[hh], Gm], [m1], out=m1[:, cs], in0=yo[hh][:, :], in1=Gm[:, cs], op=ALU.mult)
        op("dve", "tensor_tensor", [m1, xtile], [xtile], out=xtile[:], in0=m1[:], in1=xtile[:], op=ALU.add)
        fw.dma("sp", xacc_d[ti * 128:(ti + 1) * 128, :], xtile[:], reads=[xtile], writes=[xacc_tok])
        yield
        st = z.st[1]
        op("act", "activation", [xtile], [big, st], out=big[:], in_=xtile[:], func=AF.Square, accum_out=st[:, 0:1])
        rstd_from_ss(st[:, 0:1], D, st[:, 1:2], [st], [st])
        op("dve", "scalar_tensor_tensor", [xtile, st, Af], [m1], out=m1[:], in0=xtile[:], scalar=st[:, 1:2], in1=Af[:],
           op0=ALU.mult, op1=ALU.mult)
        op("dve", "tensor_tensor", [m1, Bf], [big], out=big[:], in0=m1[:], in1=Bf[:], op=ALU.add)
        fw.dma("sp", h2_d[ti * 128:(ti + 1) * 128, :], big[:], reads=[big], writes=[h2_tok])
        yield
        bks = [fw.bank(), fw.bank()]
        for k in range(KD):
            v = bks[k // 4][:, :].rearrange("p (a b) -> p a b", a=4)
            op("pe", "transpose", [m1, cst], [bks[k // 4]], v[:, k % 4, :], m1[:, k * 128:(k + 1) * 128], ident)
        h2T = m1[:].rearrange("p (k t) -> p k t", k=KD)
        for k in range(KD):
            v = bks[k // 4][:, :].rearrange("p (a b) -> p a b", a=4)
            op("act", "activation", [bks[k // 4], colv], [m1], out=h2T[:, k, :], in_=v[:, k % 4, :], func=AF.Identity,
               bias=colv[:, 0, 4, k:k + 1])
        bl_ = fw.bank()
        for k in range(KD):
            op("pe", "matmul", [m1, rw], [bl_], bl_[:, 0:NE], lhsT=h2T[:, k, :], rhs=rw[:, k, :],
               start=(k == 0), stop=(k == KD - 1))
        op("dve", "tensor_reduce", [bl_], [st], out=st[:, 2:3], in_=bl_[:, 0:NE], axis=AX.X, op=ALU.max)
        op("dve", "tensor_scalar", [st], [st], out=st[:, 3:4], in0=st[:, 2:3], scalar1=-1.0, scalar2=None, op0=ALU.mult)
        op("act", "activation", [bl_, st], [z.lg, st], out=z.lg[:], in_=bl_[:, 0:NE], func=AF.Exp, bias=st[:, 3:4],
           accum_out=st[:, 4:5])
        op("dve", "reciprocal", [st], [st], out=st[:, 5:6], in_=st[:, 4:5])
        op("dve", "tensor_scalar", [z.lg, st], [AFF], out=AFF[:, ti, :], in0=z.lg[:], scalar1=st[:, 5:6], scalar2=None,
           op0=ALU.mult)
        yield "H"

    def p3_wrap(z, ti):
        first = True
        for m in body_p3(z, ti):
            yield "H"

    fw.begin_defer()
    pipeline([p3_wrap(setsC[ti % NSC], ti) for ti in range(NT)], NSC)
    fw.flush(WIN)

    if debug == "xmid":
        fw.dma("sp", aff_dbg.ap(), AFF[:], reads=[AFF], writes=[xacc_tok], sem=fw.new_dsem("affdbg"))
        fw.finish([xacc_tok, h2_tok])
        print("ins", fw.n_ins, "waits", fw.n_wait, fw.seq)
        return nc

    barrier()
    S1.close()

    S2 = ExitStack()
    M = lambda name, shape, dtp, dma=False, stack=S2: fw.sb(name, shape, dtp, dma=dma, stack=stack)
    wgp = [M("wg%d" % i, [128, KD, 512], BF16, dma=True) for i in range(4)]
    wup = [M("wu%d" % i, [128, KD, 512], BF16, dma=True) for i in range(4)]
    wdp = [M("wd%d" % i, [128, 16, 512], BF16, dma=True) for i in range(2)]
    idx_all = M("idx_all", [128, NE, SB], I32, dma=True)
    gate_all = M("gate_all", [128, NE, SB], F32)
    gf_bc = M("gf_bc", [128, D], F32, dma=True)
    fw.dma("sp", gf_bc[:], mods_d[0, 5:6, :].to_broadcast([128, D]), reads=[mods_tok], writes=[gf_bc])

    def load_expert(e, parts=(0, 1, 2, 3)):
        for i in parts:
            fw.dma("pool", wgp[i][:], wg_l[e][:, i * 512:(i + 1) * 512].rearrange("(k p) n -> p k n", p=128),
                   writes=[wgp[i]])
            fw.dma("pool", wup[i][:], wu_l[e][:, i * 512:(i + 1) * 512].rearrange("(k p) n -> p k n", p=128),
                   writes=[wup[i]])

    def load_expert_down(e):
        for i in range(2):
            fw.dma("pool", wdp[i][:], wd_l[e][:, i * 512:(i + 1) * 512].rearrange("(k p) n -> p k n", p=128),
                   writes=[wdp[i]])

    if debug != "route":
        load_expert(0)
        load_expert_down(0)

    with ExitStack() as SR:
        R = lambda name, shape, dtp: fw.sb(name, shape, dtp, stack=SR)
        NTE = NT * NE
        cst = fw.sb("cstB", [128, NCST], F32, dma=True, stack=SR)
        fw.dma("sp", cst[:], cst_d.ap(), writes=[cst])
        ones = cst[:, C_ONES:C_ONES + 128]
        lo = R("lo", [128, NE], F32)
        hi = R("hi", [128, NE], F32)
        mid = R("mid", [128, NE], F32)
        cnt = R("cnt", [128, NE], F32)
        ge = R("ge", [128, NE], F32)
        dd = R("dd", [128, NE], F32)
        cmp_ = R("cmp", [128, NT, NE], F32)
        op("dve", "memset", [], [lo], lo[:], 0.0)
        op("dve", "memset", [], [hi], hi[:], 1.0)
        for it in range(n_bisect):
            op("dve", "tensor_tensor", [lo, hi], [mid], out=mid[:], in0=lo[:], in1=hi[:], op=ALU.add)
            op("dve", "tensor_scalar", [mid], [mid], out=mid[:], in0=mid[:], scalar1=0.5, scalar2=None, op0=ALU.mult)
            op("dve", "tensor_tensor", [AFF, mid], [cmp_], out=cmp_[:], in0=AFF[:],
               in1=mid[:].unsqueeze(1).to_broadcast([128, NT, NE]), op=ALU.is_ge)
            op("dve", "tensor_reduce", [cmp_], [cnt], out=cnt[:], in_=cmp_[:].rearrange("p j e -> p e j"), axis=AX.X,
               op=ALU.add)
            bk = fw.bank()
            op("pe", "matmul", [cst, cnt], [bk], bk[:, 0:NE], lhsT=ones, rhs=cnt[:], start=True, stop=True)
            op("dve", "tensor_scalar", [bk], [ge], out=ge[:], in0=bk[:, 0:NE], scalar1=float(CAP) - 0.5, scalar2=None,
               op0=ALU.is_ge)
            op("dve", "tensor_tensor", [mid, ge], [dd], out=dd[:], in0=mid[:], in1=ge[:], op=ALU.mult)
            op("dve", "tensor_tensor", [lo, dd], [lo], out=lo[:], in0=lo[:], in1=dd[:], op=ALU.max)
            op("dve", "tensor_tensor", [mid, ge], [dd], out=dd[:], in0=mid[:], in1=ge[:], op=ALU.add)
            op("dve", "tensor_tensor", [hi, dd], [hi], out=hi[:], in0=hi[:], in1=dd[:], op=ALU.min)
        sel = R("sel", [128, NT, NE], F32)
        op("dve", "tensor_tensor", [AFF, lo], [sel], out=sel[:], in0=AFF[:],
           in1=lo[:].unsqueeze(1).to_broadcast([128, NT, NE]), op=ALU.is_ge)
        self2 = sel[:].rearrange("p j e -> p (j e)")
        pos = R("pos", [128, NT, NE], F32)
        pos2 = pos[:].rearrange("p j e -> p (j e)")
        tot = R("tot", [128, NT, NE], F32)
        tot2 = tot[:].rearrange("p j e -> p (j e)")
        cA = R("cA", [128, NT, NE], F32)
        cB = R("cB", [128, NT, NE], F32)
        lstr = cst[:, C_LSTR:C_LSTR + 128]
        ncol = (NTE + 511) // 512
        for c in range(ncol):
            c0, c1 = c * 512, min(NTE, (c + 1) * 512)
            bk = fw.bank()
            op("pe", "matmul", [cst, sel], [bk], bk[:, 0:c1 - c0], lhsT=lstr, rhs=self2[:, c0:c1], start=True, stop=True)
            op("dve", "tensor_copy", [bk], [pos], out=pos2[:, c0:c1], in_=bk[:, 0:c1 - c0])
            bk = fw.bank()
            op("pe", "matmul", [cst, sel], [bk], bk[:, 0:c1 - c0], lhsT=ones, rhs=self2[:, c0:c1], start=True, stop=True)
            op("dve", "tensor_copy", [bk], [tot], out=tot2[:, c0:c1], in_=bk[:, 0:c1 - c0])
        src = tot
        op("dve", "tensor_copy", [tot], [cA], out=cA[:], in_=tot[:])
        cur, nxt = cA, cB
        s_ = 1
        while s_ < NT:
            op("dve", "tensor_tensor", [cur], [nxt], out=nxt[:, s_:, :], in0=cur[:, s_:, :], in1=cur[:, :NT - s_, :],
               op=ALU.add)
            op("dve", "tensor_copy", [cur], [nxt], out=nxt[:, :s_, :], in_=cur[:, :s_, :])
            cur, nxt = nxt, cur
            s_ *= 2
        op("dve", "tensor_tensor", [cur, tot], [cur], out=cur[:], in0=cur[:], in1=tot[:], op=ALU.subtract)
        op("dve", "tensor_tensor", [pos, cur], [pos], out=pos[:], in0=pos[:], in1=cur[:], op=ALU.add)
        op("dve", "scalar_tensor_tensor", [pos, sel], [pos], out=pos2, in0=pos2, scalar=1.0, in1=self2,
           op0=ALU.add, op1=ALU.mult)
        op("dve", "tensor_scalar", [pos], [pos], out=pos[:], in0=pos[:], scalar1=-1.0, scalar2=None, op0=ALU.add)
        pdiv = nxt
        op("dve", "memset", [], [pdiv], pdiv[:], 0.0)
        pdiv2 = pdiv[:].rearrange("p j e -> p (j e)")
        for b_ in range(1, SB):
            op("dve", "scalar_tensor_tensor", [pos, pdiv], [pdiv], out=pdiv2, in0=pos2, scalar=128.0 * b_ - 0.5,
               in1=pdiv2, op0=ALU.is_ge, op1=ALU.add)
        pmod = cur
        pmod2 = pmod[:].rearrange("p j e -> p (j e)")
        op("dve", "scalar_tensor_tensor", [pdiv, pos], [pmod], out=pmod2, in0=pdiv2, scalar=-128.0, in1=pos2,
           op0=ALU.mult, op1=ALU.add)
        ahi = R("ahi", [128, NT, NE], BF16)
        alo = R("alo", [128, NT, NE], BF16)
        op("dve", "tensor_copy", [AFF], [ahi], out=ahi[:], in_=AFF[:])
        op("dve", "tensor_tensor", [AFF, ahi], [alo], out=alo[:], in0=AFF[:], in1=ahi[:], op=ALU.subtract)
        Be2 = [R("Be%d" % i, [128, NT, 128], BF16) for i in range(2)]
        Ae2 = [R("Ae%d" % i, [128, NT, 8], F32) for i in range(2)]
        Re2 = [R("Re%d" % i, [128, NT, 4, 8], BF16) for i in range(2)]
        iosi = cst[:, C_IOSI:C_IOSI + 128]
        iosb = cst[:, C_IOSB:C_IOSB + 8]
        jv = cst[:, C_JV:C_JV + NT]
        tokf = R("tokf", [128, SB], F32)
        rsb = R("rsb", [128, 32], F32)
        for e in range(NE):
            Be, Ae, Re = Be2[e % 2], Ae2[e % 2], Re2[e % 2]
            for j in range(NT):
                op("dve", "tensor_scalar", [cst, pmod], [Be], out=Be[:, j, :], in0=iosi, scalar1=pmod[:, j, e:e + 1],
                   scalar2=None, op0=ALU.is_equal, same_slice=(j > 0))
            op("dve", "tensor_tensor", [cst, pdiv], [Ae], out=Ae[:], in0=iosb.unsqueeze(1).to_broadcast([128, NT, 8]),
               in1=pdiv[:, :, e:e + 1].to_broadcast([128, NT, 8]), op=ALU.is_equal)
            op("dve", "tensor_scalar", [Ae, cst], [Re], out=Re[:, :, 0, :], in0=Ae[:], scalar1=cst[:, C_PV:C_PV + 1],
               scalar2=None, op0=ALU.mult)
            op("dve", "tensor_tensor", [Ae, cst], [Re], out=Re[:, :, 1, :], in0=Ae[:],
               in1=jv.unsqueeze(2).to_broadcast([128, NT, 8]), op=ALU.mult)
            op("dve", "tensor_tensor", [Ae, ahi], [Re], out=Re[:, :, 2, :], in0=Ae[:],
               in1=ahi[:, :, e:e + 1].to_broadcast([128, NT, 8]), op=ALU.mult)
            op("dve", "tensor_tensor", [Ae, alo], [Re], out=Re[:, :, 3, :], in0=Ae[:],
               in1=alo[:, :, e:e + 1].to_broadcast([128, NT, 8]), op=ALU.mult)
            bk = fw.bank()
            for j in range(NT):
                op("pe", "matmul", [Be, Re], [bk], bk[:, 0:32], lhsT=Be[:, j, :],
                   rhs=Re[:, j, :, :].rearrange("p c s -> p (c s)"), start=(j == 0), stop=(j == NT - 1))
            op("dve", "tensor_copy", [bk], [rsb], out=rsb[:], in_=bk[:, 0:32])
            op("dve", "scalar_tensor_tensor", [rsb], [tokf], out=tokf[:], in0=rsb[:, 8:8 + SB], scalar=128.0,
               in1=rsb[:, 0:SB], op0=ALU.mult, op1=ALU.add)
            op("dve", "tensor_scalar", [tokf], [tokf], out=tokf[:], in0=tokf[:], scalar1=-1.0, scalar2=None, op0=ALU.add)
            op("dve", "tensor_copy", [tokf], [idx_all], out=idx_all[:, e, :], in_=tokf[:])
            op("dve", "tensor_tensor", [rsb], [gate_all], out=gate_all[:, e, :], in0=rsb[:, 16:16 + SB],
               in1=rsb[:, 24:24 + SB], op=ALU.add)
        if debug == "route":
            dbg_tok = fw.token("dbg")
            fw.dma("sp", idx_dbg.ap(), idx_all[:], reads=[idx_all], writes=[dbg_tok], sem=idx_all.dsem)
            fw.dma("sp", aff_dbg.ap(), AFF[:], reads=[AFF], writes=[dbg_tok], sem=fw.new_dsem("affdbg"))
            gsem = fw.new_dsem("gdbg")
            fw.dma("sp", gate_dbg.ap(), gate_all[:], reads=[gate_all], writes=[dbg_tok], sem=gsem)
            fw.finish([dbg_tok, xacc_tok, h2_tok])
            print("ins", fw.n_ins, "waits", fw.n_wait, fw.seq)
            return nc
        barrier()

    xg = [M("xg%d" % i, [128, D], BF16, dma=True) for i in range(4)]
    xsT2 = [M("xsT%d" % i, [128, KD, CAP], BF16) for i in range(2)]
    hidT = M("hidT", [128, 16, CAP], BF16)
    sgt = M("sgt", [128, 512], F32)
    yo_ = [M("yo%d" % i, [128, D], F32, dma=True) for i in range(2)]
    scat_tok = [fw.token("scat0"), fw.token("scat1")]
    NS = (CAP + 511) // 512
    GA = list(range((SB + 1) // 2))
    GB = list(range((SB + 1) // 2, SB))

    def issue_gathers(e, sbs):
        for sb_ in sbs:
            g_ = xg[sb_ % 4]
            fw.dma("pool", g_[:], h2_d[:, :], reads=[h2_tok, idx_all], writes=[g_],
                   indirect=dict(out_offset=None,
                                 in_offset=bass.IndirectOffsetOnAxis(ap=idx_all[:, e, sb_:sb_ + 1], axis=0)))

    def do_transposes(e, sbs):
        xs = xsT2[e % 2]
        for sb_ in sbs:
            g_ = xg[sb_ % 4]
            bk = fw.bank()
            v = bk[:].bitcast(BF16).rearrange("p (a b) -> p a b", a=8)
            for k in range(KD):
                op("pe", "transpose", [g_, identb], [bk], v[:, k, :], g_[:, k * 128:(k + 1) * 128], identb[:])
            op("act", "activation", [bk], [xs], out=xs[:, :, sb_ * 128:(sb_ + 1) * 128], in_=v, func=AF.Copy)

    issue_gathers(0, GA)
    do_transposes(0, GA)
    issue_gathers(0, GB)
    do_transposes(0, GB)
    for e in range(NE):
        xsT = xsT2[e % 2]
        more = e + 1 < NE
        for fq in range(4):
            for fc in range(4):
                f_ = fq * 4 + fc
                for s in range(NS):
                    s0, s1 = s * 512, min(CAP, (s + 1) * 512)
                    bg_ = fw.bank()
                    for k in range(KD):
                        op("pe", "matmul", [wgp[fq], xsT], [bg_], bg_[:, 0:s1 - s0],
                           lhsT=wgp[fq][:, k, fc * 128:(fc + 1) * 128], rhs=xsT[:, k, s0:s1],
                           start=(k == 0), stop=(k == KD - 1))
                    bu_ = fw.bank()
                    for k in range(KD):
                        op("pe", "matmul", [wup[fq], xsT], [bu_], bu_[:, 0:s1 - s0],
                           lhsT=wup[fq][:, k, fc * 128:(fc + 1) * 128], rhs=xsT[:, k, s0:s1],
                           start=(k == 0), stop=(k == KD - 1))
                    op("act", "activation", [bg_], [sgt], out=sgt[:, 0:s1 - s0], in_=bg_[:, 0:s1 - s0], func=AF.Sigmoid)
                    op("dve", "tensor_tensor", [bg_, sgt], [sgt], out=sgt[:, 0:s1 - s0], in0=bg_[:, 0:s1 - s0],
                       in1=sgt[:, 0:s1 - s0], op=ALU.mult)
                    op("dve", "tensor_tensor", [bu_, sgt], [hidT], out=hidT[:, f_, s0:s1], in0=bu_[:, 0:s1 - s0],
                       in1=sgt[:, 0:s1 - s0], op=ALU.mult)
            if more:
                load_expert(e + 1, (fq,))
        if more:
            issue_gathers(e + 1, GA)
        for sb_ in range(SB):
            if more and sb_ == SB // 2:
                do_transposes(e + 1, GA)
                issue_gathers(e + 1, GB)
            y_ = yo_[sb_ % 2]
            for hh in range(2):
                bd = fw.bank()
                for f_ in range(16):
                    op("pe", "matmul", [hidT, wdp[hh]], [bd], bd[:, :], lhsT=hidT[:, f_, sb_ * 128:(sb_ + 1) * 128],
                       rhs=wdp[hh][:, f_, :], start=(f_ == 0), stop=(f_ == 15))
                op("dve", "scalar_tensor_tensor", [bd, gate_all, gf_bc], [y_], out=y_[:, hh * 512:(hh + 1) * 512],
                   in0=bd[:, :], scalar=gate_all[:, e, sb_:sb_ + 1], in1=gf_bc[:, hh * 512:(hh + 1) * 512],
                   op0=ALU.mult, op1=ALU.mult)
            fw.dma("pool", xacc_d[:, :], y_[:], reads=[y_, idx_all], writes=[scat_tok[e % 2]],
                   extra=[scat_tok[(e + 1) % 2], xacc_tok], nowait_w=True,
                   indirect=dict(out_offset=bass.IndirectOffsetOnAxis(ap=idx_all[:, e, sb_:sb_ + 1], axis=0),
                                 in_offset=None, compute_op=ALU.add))
        if more:
            load_expert_down(e + 1)
            do_transposes(e + 1, GB)

    barrier()
    S2.close()
    S3 = ExitStack()
    xf = [fw.sb("xf%d" % i, [128, D], F32, dma=True, stack=S3) for i in range(4)]
    of = [fw.sb("of%d" % i, [128, D], F32, dma=True, stack=S3) for i in range(4)]
    jk = fw.sb("jk", [128, D], BF16, stack=S3)
    stf = fw.sb("stf", [128, 4], F32, stack=S3)
    eps2 = fw.sb("eps2", [128, 1], F32, stack=S3)
    finw = fw.sb("finw", [128, D], F32, dma=True, stack=S3)
    fw.dma("sp", finw[:], nw_d[2:3, :].to_broadcast([128, D]), writes=[finw])
    op("pool", "memset", [], [eps2], eps2[:], EPS)
    out_tok = fw.token("out_tok")
    for ti in range(NT):
        x_ = xf[ti % 4]
        o_ = of[ti % 4]
        fw.dma("sp" if ti % 2 == 0 else "pool", x_[:], xacc_d[ti * 128:(ti + 1) * 128, :], reads=[xacc_tok, scat_tok[0], scat_tok[1]], writes=[x_])
        op("act", "activation", [x_], [jk, stf], out=jk[:], in_=x_[:], func=AF.Square, accum_out=stf[:, 0:1])
        op("act", "activation", [stf, eps2], [stf], out=stf[:, 1:2], in_=stf[:, 0:1], func=AF.Ln, scale=1.0 / D,
           bias=eps2[:, 0:1])
        op("act", "activation", [stf], [stf], out=stf[:, 1:2], in_=stf[:, 1:2], func=AF.Exp, scale=-0.5)
        op("dve", "scalar_tensor_tensor", [x_, stf, finw], [o_], out=o_[:], in0=x_[:], scalar=stf[:, 1:2], in1=finw[:],
           op0=ALU.mult, op1=ALU.mult)
        fw.dma("act", out_d[ti * 128:(ti + 1) * 128, :], o_[:], reads=[o_], writes=[out_tok])
    fw.finish([out_tok])
    print("ins", fw.n_ins, "waits", fw.n_wait, fw.seq)
    return nc


def make_in_map(b, x, c, ctx, c_ctx, ada_w, ada_b, norm_mix_w, norm_ffn_w, w_in, hgrn_lb_logits,
                hgrn_norm_w, sgu_norm_w, sgu_w, sgu_b, w_branch_a, w_branch_b, w_out, router_w,
                expert_w_gate, expert_w_up, expert_w_down, final_norm_w, T=None):
    f = lambda a: np.ascontiguousarray(np.asarray(a, dtype=np.float32))
    cc = np.stack([np.asarray(c)[b], np.asarray(c_ctx)], axis=0)
    ccT = cc.reshape(2, KD, 128).transpose(2, 1, 0)
    xb = np.asarray(x)[b]
    if T is not None:
        xb = xb[:T]
    return {
        "x": f(xb), "ctx": f(np.asarray(ctx)[b]), "ccT": f(ccT),
        "ada_w": f(np.asarray(ada_w)[0]), "ada_b": f(np.asarray(ada_b)[0].reshape(1, -1)),
        "nw": f(np.stack([np.asarray(norm_mix_w)[0], np.asarray(norm_ffn_w)[0], np.asarray(final_norm_w)], 0)),
        "w_in": f(np.asarray(w_in)[0]),
        "lbl": f(np.asarray(hgrn_lb_logits)[:, 0:2, :].reshape(1, -1)),
        "hnw": f(np.asarray(hgrn_norm_w)[0].reshape(1, -1)), "snw": f(np.asarray(sgu_norm_w)[0].reshape(1, -1)),
        "sgu_wT": f(np.asarray(sgu_w)[0].transpose(2, 0, 1)),
        "sgu_bT": f(np.asarray(sgu_b)[0].T),
        "w_a": f(np.asarray(w_branch_a)[0]), "w_b": f(np.asarray(w_branch_b)[0]), "w_o": f(np.asarray(w_out)[0]),
        "rw": f(np.asarray(router_w)[0]),
        "cst": make_consts(),
        **{"wg%d" % e: f(np.asarray(expert_w_gate)[0, e]) for e in range(NE)},
        **{"wu%d" % e: f(np.asarray(expert_w_up)[0, e]) for e in range(NE)},
        **{"wd%d" % e: f(np.asarray(expert_w_down)[0, e]) for e in range(NE)},
    }


def kernel(**inputs):
    B = np.asarray(inputs["x"]).shape[0]
    nc = build(64)
    n = 8
    real_cores = [0, 1, 4, 5][:B]
    maps = [make_in_map(b, **inputs) for b in range(B)]
    for i in range(1, B):
        for k in maps[0]:
            if k not in ("x", "ctx", "ccT"):
                maps[i][k] = maps[0][k]
    dummy = {k: (v if k == "cst" else np.zeros_like(v)) for k, v in maps[0].items()}
    in_maps = []
    for c in range(n):
        in_maps.append(maps[real_cores.index(c)] if c in real_cores else dummy)
    res = run_bass_kernel_spmd(nc, in_maps, core_ids=list(range(n)))
    out = np.stack([np.asarray(res.results[real_cores[b]]["out"]) for b in range(B)], axis=0)
    return out.astype(np.float32)
```
